# Optimizing a Trainium2 kernel written in Bass

```python
import math
import jax, jax.numpy as jnp
from jax import lax
import numpy as np

D_MODEL = 1024
BATCH = 4
SEQ = 8192
DEPTH = 2

CHUNK = 64
Q_BLOCK = 128
D_MIX = D_MODEL
LRU_WIDTH = D_MIX // 2
LRU_BLOCKS = 8
LRU_BLOCK_W = LRU_WIDTH // LRU_BLOCKS
CONV_W = 4
RG_C = 8.0
ATTN_WIDTH = D_MIX - LRU_WIDTH
DIFF_HEADS = 4
DIFF_VDIM = ATTN_WIDTH // DIFF_HEADS
DIFF_DH = DIFF_VDIM // 2
ROPE_THETA = 10000.0
D_IN = 2 * LRU_WIDTH + 3 * ATTN_WIDTH
D_FF = 3 * D_MODEL
N_EXPERTS = 8
TOP_K = 2
N_DENSE = (DEPTH + 1) // 2
N_MOE = DEPTH // 2
EPS = 1e-6
MAX_OFFSET_CHUNKS = 64

kernel_name = 'hybrid_rglru_diffattn_moe_adaln'


def rmsnorm(x, g):
    x32 = x.astype(jnp.float32)
    y = x32 * lax.rsqrt(jnp.mean(x32 * x32, axis=-1, keepdims=True) + EPS)
    return y.astype(x.dtype) * g


def rope5(x, pos):
    inv = ROPE_THETA ** (-jnp.arange(0, DIFF_DH, 2, dtype=jnp.float32) / DIFF_DH)
    ang = pos.astype(jnp.float32)[..., None] * inv
    ang = jnp.concatenate([ang, ang], axis=-1)[:, :, None, None, :]
    cos, sin = jnp.cos(ang), jnp.sin(ang)
    x1, x2 = jnp.split(x, 2, axis=-1)
    rot = jnp.concatenate([-x2, x1], axis=-1)
    return (x * cos + rot * sin).astype(x.dtype)


def causal_conv(x, w, b):
    rhs = w[:, None, :]
    y = lax.conv_general_dilated(x, rhs, window_strides=(1,), padding=[(CONV_W - 1, 0)],
                                 dimension_numbers=('NWC', 'WIO', 'NWC'),
                                 feature_group_count=x.shape[-1])
    return y + b


def _lin_rec(e1, e2):
    a1, b1 = e1
    a2, b2 = e2
    return a1 * a2, a2 * b1 + b2


def rg_lru(x, wa, ba, wx, bx, lam):
    B, S, W = x.shape
    xb = x.reshape(B, S, LRU_BLOCKS, LRU_BLOCK_W)
    r = jax.nn.sigmoid(jnp.einsum('bsnh,nhk->bsnk', xb, wa).reshape(B, S, W) + ba)
    i = jax.nn.sigmoid(jnp.einsum('bsnh,nhk->bsnk', xb, wx).reshape(B, S, W) + bx)
    log_a = -RG_C * r.astype(jnp.float32) * jax.nn.softplus(-lam.astype(jnp.float32))
    a = jnp.exp(log_a)
    mult = jnp.sqrt(-jnp.expm1(2.0 * log_a))
    bt = mult * (i * x).astype(jnp.float32)
    _, h = lax.associative_scan(_lin_rec, (a, bt), axis=1)
    return h.astype(x.dtype)


def diff_attention(q, k, v, pos, lq1, lk1, lq2, lk2, subln_g, lambda_init):
    B, S, _ = q.shape
    q = rope5(q.reshape(B, S, DIFF_HEADS, 2, DIFF_DH), pos)
    k = rope5(k.reshape(B, S, DIFF_HEADS, 2, DIFF_DH), pos)
    v = v.reshape(B, S, DIFF_HEADS, DIFF_VDIM)
    lam = (jnp.exp(jnp.sum(lq1.astype(jnp.float32) * lk1.astype(jnp.float32)))
           - jnp.exp(jnp.sum(lq2.astype(jnp.float32) * lk2.astype(jnp.float32)))
           + lambda_init)
    chunk = pos // CHUNK
    nqb = S // Q_BLOCK
    qs = q.reshape(B, nqb, Q_BLOCK, DIFF_HEADS, 2, DIFF_DH).transpose(1, 0, 3, 4, 2, 5)
    qc = chunk.reshape(B, nqb, Q_BLOCK).transpose(1, 0, 2)
    kt = k.transpose(0, 2, 3, 1, 4)
    vt = v.transpose(0, 2, 1, 3)
    scale = DIFF_DH ** -0.5
    neg = jnp.finfo(jnp.float32).min

    def attend(args):
        qb, qcb = args
        s = jnp.einsum('bhcqd,bhckd->bhcqk', qb, kt).astype(jnp.float32) * scale
        mask = (chunk[:, None, :] <= qcb[:, :, None])[:, None, None]
        p = jax.nn.softmax(jnp.where(mask, s, neg), axis=-1)
        pd = p[:, :, 0] - lam * p[:, :, 1]
        return jnp.einsum('bhqk,bhkd->bhqd', pd.astype(vt.dtype), vt)

    o = lax.map(attend, (qs, qc))
    o = o.transpose(1, 0, 3, 2, 4).reshape(B, S, DIFF_HEADS, DIFF_VDIM)
    o = rmsnorm(o, subln_g) * (1.0 - lambda_init)
    return o.reshape(B, S, ATTN_WIDTH)


def swiglu(h, wg, wu, wd):
    return (jax.nn.silu(h @ wg) * (h @ wu)) @ wd


def moe(h, router, wg, wu, wd):
    logits = (h @ router).astype(jnp.float32)
    vals, idx = lax.top_k(logits, TOP_K)
    w = jax.nn.softmax(vals, axis=-1)
    gates = jnp.sum(jax.nn.one_hot(idx, N_EXPERTS, dtype=jnp.float32) * w[..., None], axis=-2)
    y = jnp.zeros_like(h)
    for e in range(N_EXPERTS):
        y = y + gates[..., e:e + 1].astype(h.dtype) * swiglu(h, wg[e], wu[e], wd[e])
    return y


def setup_inputs(seed: int = 0) -> dict:
    key = jax.random.key(seed)
    ks = iter(jax.random.split(key, 40))
    f32 = jnp.float32

    def nrm(shape, scale):
        return jax.random.normal(next(ks), shape, f32) * scale

    a0 = jax.random.uniform(next(ks), (DEPTH, LRU_WIDTH), f32, 0.9, 0.999)
    s0 = a0 ** (1.0 / RG_C)
    lru_lambda = jnp.log(s0) - jnp.log1p(-s0)
    offsets = jax.random.randint(next(ks), (BATCH, 1), 0, MAX_OFFSET_CHUNKS) * CHUNK
    positions = (offsets + jnp.arange(SEQ, dtype=jnp.int32)[None, :]).astype(jnp.int32)
    return {
        'x': nrm((BATCH, SEQ, D_MODEL), 1.0),
        'c': nrm((BATCH, D_MODEL), 1.0),
        'positions': positions,
        'ada_w': nrm((DEPTH, D_MODEL, 6 * D_MODEL), 0.5 * D_MODEL ** -0.5),
        'ada_b': nrm((DEPTH, 6 * D_MODEL), 0.02),
        'ln1_g': 1.0 + nrm((DEPTH, D_MODEL), 0.02),
        'ln2_g': 1.0 + nrm((DEPTH, D_MODEL), 0.02),
        'w_in': nrm((DEPTH, D_MODEL, D_IN), D_MODEL ** -0.5),
        'conv_w': nrm((DEPTH, CONV_W, LRU_WIDTH), CONV_W ** -0.5),
        'conv_b': nrm((DEPTH, LRU_WIDTH), 0.02),
        'gate_a_w': nrm((DEPTH, LRU_BLOCKS, LRU_BLOCK_W, LRU_BLOCK_W), LRU_BLOCK_W ** -0.5),
        'gate_a_b': nrm((DEPTH, LRU_WIDTH), 0.02),
        'gate_x_w': nrm((DEPTH, LRU_BLOCKS, LRU_BLOCK_W, LRU_BLOCK_W), LRU_BLOCK_W ** -0.5),
        'gate_x_b': nrm((DEPTH, LRU_WIDTH), 0.02),
        'lru_lambda': lru_lambda,
        'lam_q1': nrm((DEPTH, DIFF_DH), 0.1),
        'lam_k1': nrm((DEPTH, DIFF_DH), 0.1),
        'lam_q2': nrm((DEPTH, DIFF_DH), 0.1),
        'lam_k2': nrm((DEPTH, DIFF_DH), 0.1),
        'subln_g': 1.0 + nrm((DEPTH, DIFF_VDIM), 0.02),
        'w_out': nrm((DEPTH, D_MIX, D_MODEL), D_MIX ** -0.5),
        'ffn_w_gate': nrm((N_DENSE, D_MODEL, D_FF), D_MODEL ** -0.5),
        'ffn_w_up': nrm((N_DENSE, D_MODEL, D_FF), D_MODEL ** -0.5),
        'ffn_w_down': nrm((N_DENSE, D_FF, D_MODEL), D_FF ** -0.5),
        'moe_router': nrm((N_MOE, D_MODEL, N_EXPERTS), D_MODEL ** -0.5),
        'moe_w_gate': nrm((N_MOE, N_EXPERTS, D_MODEL, D_FF), D_MODEL ** -0.5),
        'moe_w_up': nrm((N_MOE, N_EXPERTS, D_MODEL, D_FF), D_MODEL ** -0.5),
        'moe_w_down': nrm((N_MOE, N_EXPERTS, D_FF, D_MODEL), D_FF ** -0.5),
        'final_g': 1.0 + nrm((D_MODEL,), 0.02),
    }


def reference(x, c, positions, ada_w, ada_b, ln1_g, ln2_g, w_in, conv_w, conv_b,
              gate_a_w, gate_a_b, gate_x_w, gate_x_b, lru_lambda, lam_q1, lam_k1,
              lam_q2, lam_k2, subln_g, w_out, ffn_w_gate, ffn_w_up, ffn_w_down,
              moe_router, moe_w_gate, moe_w_up, moe_w_down, final_g):
    splits = [LRU_WIDTH, 2 * LRU_WIDTH, 2 * LRU_WIDTH + ATTN_WIDTH, 2 * LRU_WIDTH + 2 * ATTN_WIDTH]
    for l in range(DEPTH):
        lambda_init = 0.8 - 0.6 * math.exp(-0.3 * l)
        mod = jax.nn.silu(c) @ ada_w[l] + ada_b[l]
        sh1, sc1, g1, sh2, sc2, g2 = jnp.split(mod[:, None, :], 6, axis=-1)
        h = rmsnorm(x, ln1_g[l]) * (1.0 + sc1) + sh1
        z = h @ w_in[l]
        xr, yr, q, k, v = jnp.split(z, splits, axis=-1)
        lru = rg_lru(causal_conv(xr, conv_w[l], conv_b[l]), gate_a_w[l], gate_a_b[l],
                     gate_x_w[l], gate_x_b[l], lru_lambda[l]) * jax.nn.gelu(yr)
        att = diff_attention(q, k, v, positions, lam_q1[l], lam_k1[l], lam_q2[l], lam_k2[l],
                             subln_g[l], lambda_init)
        x = x + g1 * (jnp.concatenate([lru, att], axis=-1) @ w_out[l])
        h = rmsnorm(x, ln2_g[l]) * (1.0 + sc2) + sh2
        if l % 2 == 0:
            j = l // 2
            f = swiglu(h, ffn_w_gate[j], ffn_w_up[j], ffn_w_down[j])
        else:
            j = l // 2
            f = moe(h, moe_router[j], moe_w_gate[j], moe_w_up[j], moe_w_down[j])
        x = x + g2 * f
    return rmsnorm(x, final_g)
```

```python
import contextlib
import math
import numpy as np
import ml_dtypes
import concourse.bass as bass
import concourse.mybir as mybir
from concourse.bass_utils import run_bass_kernel_spmd

F32 = mybir.dt.float32
BF16 = mybir.dt.bfloat16
I32 = mybir.dt.int32
AF = mybir.ActivationFunctionType
ALU = mybir.AluOpType
AX = mybir.AxisListType

D = 1024
S = 8192
NB = 4
DEPTH = 2
DFF = 3072
NE = 8
EPS = 1e-6
TT = 512
NT = S // TT
TWO_PI = 2.0 * math.pi
CW1 = 6.28125
CW2 = TWO_PI - CW1


class Tok:
    __slots__ = ("sem", "val", "eng")

    def __init__(self, sem, val, eng):
        self.sem, self.val, self.eng = sem, val, eng


class Buf:
    def __init__(self, t):
        self.t = t
        self.w = None
        self.r = {}

    def __getitem__(self, idx):
        return self.t[idx]


class Eng:
    def __init__(self, name, h, sem):
        self.name, self.h, self.sem = name, h, sem
        self.count = 0
        self.waited = {}
        self.pending = []


class B:
    def __init__(self, nc, es):
        self.nc = nc
        self.es = es
        self.eng = {}
        for name, h in (("pe", nc.tensor), ("act", nc.scalar), ("dve", nc.vector),
                        ("pool", nc.gpsimd), ("sp", nc.sync)):
            sem = es.enter_context(nc.semaphore("s_" + name))
            self.eng[name] = Eng(name, h, sem)
        self.nsem = 0
        self.out_toks = []
        self.psn = 0
        self.dsems = []
        self.pes = None
        self.ptag = ""

    def begin_phase(self, tag):
        self.ptag = tag
        self.pes = contextlib.ExitStack()
        self.pes.__enter__()

    def end_phase(self):
        self.barrier()
        self.pes.__exit__(None, None, None)
        self.pes = None

    def barrier(self):
        toks = [Tok(e.sem, e.count, e) for e in self.eng.values() if e.count > 0]
        toks += [Tok(sm[0], sm[1], None) for sm in self.dsems if sm[1] > 0]
        for e in self.eng.values():
            assert not e.pending
            for t in toks:
                if t.eng is e:
                    continue
                self._wait(e, t)

    def sb(self, name, shape, dt):
        st = self.pes if self.pes is not None else self.es
        return Buf(st.enter_context(self.nc.sbuf_tensor("sb_" + self.ptag + name, list(shape), dt)))

    def ps(self, name):
        return Buf(self.es.enter_context(self.nc.psum_tensor("ps_" + name, [128, 512], F32)))

    def dsem(self):
        self.nsem += 1
        sm = [self.es.enter_context(self.nc.semaphore("d%d" % self.nsem)), 0]
        self.dsems.append(sm)
        return sm

    def _wait(self, e, tok):
        if tok is None:
            return
        if tok.eng is e and e.name in ("pe", "sp"):
            return
        assert tok.val is not None, "unresolved token"
        k = id(tok.sem)
        if e.waited.get(k, 0) >= tok.val:
            return
        e.h.wait_ge(tok.sem, tok.val)
        e.waited[k] = tok.val

    def _deps(self, e, reads, writes):
        for bf in reads:
            self._wait(e, bf.w)
        for bf in writes:
            self._wait(e, bf.w)
            for t in bf.r.values():
                self._wait(e, t)

    def _record(self, tok, reads, writes):
        for bf in writes:
            bf.w = tok
            bf.r = {}
        for bf in reads:
            if bf not in writes:
                bf.r[id(tok.sem)] = tok

    def op(self, en, fn, reads=(), writes=(), inc=True):
        e = self.eng[en]
        self._deps(e, reads, writes)
        ins = fn(e.h)
        tok = Tok(e.sem, None, e)
        if inc:
            e.count += 1
            ins.then_inc(e.sem, 1)
            tok.val = e.count
            for p in e.pending:
                p.val = e.count
            e.pending = []
        else:
            e.pending.append(tok)
        self._record(tok, reads, writes)
        return tok

    def dma(self, qn, out, in_, reads=(), writes=(), sem=None, is_out=False, group=None):
        e = self.eng[qn]
        self._deps(e, reads, writes)
        ins = e.h.dma_start(out=out, in_=in_)
        sem[1] += 16
        ins.then_inc(sem[0], 16)
        tok = Tok(sem[0], sem[1], None)
        self._record(tok, reads, writes)
        if is_out:
            self.out_toks.append(tok)
        if group is not None:
            group.append(tok)
        return tok

    @staticmethod
    def seal(group, sem):
        for t in group:
            t.val = sem[1]
        del group[:]

    def finish(self):
        e = self.eng["sp"]
        last = {}
        for t in self.out_toks:
            last[id(t.sem)] = t
        for t in last.values():
            self._wait(e, t)


def emit_adaln(b, nc, P, adaw_d, ncols, cs_sb, modraw, adaw_slots, adaw_sems):
    nch = ncols // 128
    adv = adaw_d.rearrange("(k p) n -> p k n", p=128)
    psb = P[0]
    for j in range(nch):
        slot = adaw_slots[j % len(adaw_slots)]
        b.dma("sp", slot[:], adv[:, :, j * 128:(j + 1) * 128], writes=[slot], sem=adaw_sems[j % len(adaw_slots)])
        for kc in range(8):
            b.op("pe", lambda h, kc=kc, slot=slot, j=j: h.matmul(
                psb[:, j:j + 1], slot[:, kc, :], cs_sb[:, kc:kc + 1], start=(kc == 0), stop=(kc == 7)),
                reads=[slot, cs_sb], writes=[psb], inc=(kc == 7))
    b.op("dve", lambda h: h.tensor_copy(out=modraw[:, 0:nch], in_=psb[:, 0:nch]), reads=[psb], writes=[modraw])


def emit_mod(b, nc, P, tag, c_d, adaw_d, adab_d, modall, first):
    b.begin_phase(tag)
    sb = b.sb
    adab = sb("adab", [128, 48], F32)
    modraw = sb("modraw", [128, 48], F32)
    slots = [sb("adaw%d" % i, [128, 8, 128], F32) for i in range(4)]
    sems = [b.dsem() for _ in range(4)]
    s_small = b.dsem()
    grp = []
    if first:
        c_sb = sb("c_sb", [128, 8], F32)
        b.dma("sp", c_sb[:], c_d, writes=[c_sb], sem=s_small, group=grp)
    b.dma("sp", adab[:], adab_d, writes=[adab], sem=s_small, group=grp)
    b.seal(grp, s_small)
    if first:
        b.op("act", lambda h: h.activation(out=b.cs[:], in_=c_sb[:], func=AF.Silu), reads=[c_sb], writes=[b.cs])
    emit_adaln(b, nc, P, adaw_d, 6 * D, b.cs, modraw, slots, sems)
    b.op("dve", lambda h: h.tensor_tensor(out=modall[:], in0=modraw[:], in1=adab[:], op=ALU.add),
         reads=[modraw, adab], writes=[modall])
    b.end_phase()


def emit_norm(b, nc, xsrc, xsl, ps_stat, ones_bf, sqb, rstd, xn, gs, sh, hdst, hsl, ncol, gcol0=0):
    for c in range(8):
        sq = sqb[c % 2]
        b.op("act", lambda h, c=c, sq=sq: h.activation(out=sq[:, 0:ncol], in_=xsrc[:, c, xsl], func=AF.Square),
             reads=[xsrc], writes=[sq])
        b.op("pe", lambda h, c=c, sq=sq: h.matmul(ps_stat[:, 0:ncol], ones_bf[:, :], sq[:, 0:ncol],
                                                  start=(c == 0), stop=(c == 7)),
             reads=[sq, ones_bf], writes=[ps_stat], inc=True)
    b.op("act", lambda h: h.activation(out=rstd[:, 0:ncol], in_=ps_stat[:, 0:ncol], func=AF.Sqrt,
                                       bias=b.eps[:, 0:1], scale=1.0 / D), reads=[ps_stat], writes=[rstd])
    b.op("dve", lambda h: h.reciprocal(out=rstd[:, 0:ncol], in_=rstd[:, 0:ncol]), reads=[rstd], writes=[rstd])
    for c in range(8):
        t = xn[c % 2]
        b.op("dve", lambda h, c=c, t=t: h.tensor_tensor(out=t[:, 0:ncol], in0=xsrc[:, c, xsl], in1=rstd[:, 0:ncol],
                                                        op=ALU.mult), reads=[xsrc, rstd], writes=[t])
        if sh is not None:
            b.op("pool", lambda h, c=c, t=t: h.tensor_scalar(
                out=hdst[:, c, hsl], in0=t[:, 0:ncol], scalar1=gs[:, gcol0 + c:gcol0 + c + 1],
                scalar2=sh[:, c:c + 1], op0=ALU.mult, op1=ALU.add), reads=[t, gs, sh], writes=[hdst])
        else:
            b.op("act", lambda h, c=c, t=t: h.activation(
                out=hdst[:, c, hsl], in_=t[:, 0:ncol], func=AF.Identity, scale=gs[:, gcol0 + c:gcol0 + c + 1]),
                reads=[t, gs], writes=[hdst])


def emit_M(b, nc, P, tag, l, dm, xv, mixv, rows, ntiles=NT):
    import collections
    lambda_init = 0.8 - 0.6 * math.exp(-0.3 * l)
    lng_d, modall = dm["lng"], dm["modall"]
    win_d, convw_d, sm_d, sgn_d, gw_d, lamv_d, pos_d = (dm["win"], dm["convw"], dm["small"], dm["sgn"], dm["gw"],
                                                        dm["lamv"], dm["pos"])
    b.begin_phase(tag)
    sb = b.sb
    ones_bf = sb("ones_bf", [128, 128], BF16)
    lng = sb("lng", [128, 8], F32)
    sh1 = sb("sh1", [128, 8], F32)
    gs1 = sb("gs1", [128, 8], F32)
    convw = sb("convw", [128, 2, 4], F32)
    sm = sb("sm", [128, 10], F32)
    nba = sb("nba", [128, 4], F32)
    sgn = sb("sgn", [128, 1], F32)
    halfpi = sb("halfpi", [128, 1], F32)
    gw = sb("gw", [128, 4, 128], BF16)
    lamv = sb("lamv", [128, 256], F32)
    lamt = sb("lamt", [128, 128], F32)
    lams = sb("lams", [128, 4], F32)
    neglam = sb("neglam", [128, 1], F32)
    gsub = sb("gsub", [128, 1], F32)
    cl = sb("cl", [128, 2], F32)
    bsel = sb("bsel", [64, 2, 128], F32)
    win = sb("win", [128, 8, 1792], BF16)
    KT = [[sb("KT%d_%d" % (hd, i), [128, TT], BF16) for i in range(NT)] for hd in range(2)]
    Vc = [sb("V%d" % i, [128, 4, 256], BF16) for i in range(NT)]
    xt = sb("xt", [128, 8, TT], F32)
    sqb = [sb("sqb%d" % i, [128, TT], BF16) for i in range(2)]
    xn = [sb("xn%d" % i, [128, TT], F32) for i in range(2)]
    rstd = sb("rstd", [128, TT], F32)
    hT = sb("hT", [128, 8, TT], BF16)
    xrb = [sb("xrb%d" % i, [128, TT + 3], F32) for i in range(2)]
    gy = [sb("gy%d" % i, [128, TT], F32) for i in range(2)]
    xg = sb("xg", [128, TT], F32)
    gz = sb("gz", [128, TT], F32)
    posb = sb("posb", [128, TT], I32)
    ang = sb("ang", [128, TT], F32)
    kint = sb("kint", [128, TT], I32)
    absr = sb("absr", [128, TT], F32)
    sinT = sb("sinT", [128, TT], F32)
    cosT = sb("cosT", [128, TT], F32)
    rt1 = sb("rt1", [128, TT], F32)
    rt2 = sb("rt2", [128, TT], F32)
    qT = [[sb("qT%d_%d" % (p_, i), [128, TT], BF16) for i in range(2)] for p_ in range(2)]
    xc32 = sb("xc32", [128, TT], F32)
    xcb = sb("xcb", [128, TT], BF16)
    ra = sb("ra", [128, TT], F32)
    a2 = sb("a2", [128, TT], F32)
    igt = sb("igt", [128, TT], F32)
    hs = sb("hs", [128, TT], F32)
    hprev = [sb("hprev%d" % i, [128, 1], F32) for i in range(2)]
    et = [[sb("e%d_%d" % (c, i), [128, TT], BF16) for i in range(3)] for c in range(2)]
    rs = [sb("rs%d" % i, [128, TT], F32) for i in range(2)]
    rsum = sb("rsum", [64, TT], F32)
    o0 = sb("o0", [128, TT], F32)
    o1 = sb("o1", [128, TT], F32)
    sqo = sb("sqo", [128, TT], BF16)
    mixo = [sb("mixo%d" % i, [128, 4, TT], BF16) for i in range(2)]

    s_small = b.dsem()
    s_win = b.dsem()
    s_x = b.dsem()
    s_pos = b.dsem()
    s_outs = [b.dsem(), b.dsem()]
    FP = P[7]
    SB = [[P[0], P[1]], [P[2], P[3]]]
    po = [P[4], P[5]]
    PS = P[6]

    grp = []
    for dst, src in ((lng, lng_d), (convw, convw_d), (sm, sm_d), (sgn, sgn_d)):
        b.dma("sp", dst[:], src, writes=[dst], sem=s_small, group=grp)
    b.dma("sp", lamv[:], lamv_d[0:1, :].to_broadcast([128, 256]), writes=[lamv], sem=s_small, group=grp)
    b.seal(grp, s_small)
    b.dma("pool", gw[:], gw_d, writes=[gw], sem=s_win, group=grp)
    winv = win_d.rearrange("(k p) n -> p k n", p=128)
    for kc in range(8):
        b.dma("pool", win[:, kc, :], winv[:, kc, :], writes=[win], sem=s_win, group=grp)
    b.seal(grp, s_win)
    b.op("dve", lambda h: h.memset(ones_bf[:], 1.0), writes=[ones_bf])
    b.op("dve", lambda h: h.memset(halfpi[:], math.pi / 2), writes=[halfpi])
    b.op("dve", lambda h: h.memset(bsel[:], 0.0), writes=[bsel])
    b.op("dve", lambda h: h.memset(bsel[0:32, 0, :], 1.0 / 32), writes=[bsel])
    b.op("dve", lambda h: h.memset(bsel[32:64, 1, :], 1.0 / 32), writes=[bsel])
    for ch in range(2):
        b.op("dve", lambda h, ch=ch: h.memset(xrb[ch][:, 0:3], 0.0), writes=[xrb[ch]])
        b.op("dve", lambda h, ch=ch: h.memset(hprev[ch][:], 0.0), writes=[hprev[ch]])
    b.op("dve", lambda h: h.tensor_scalar(out=nba[:], in0=sm[:, 2:6], scalar1=-1.0, scalar2=None, op0=ALU.mult),
         reads=[sm], writes=[nba])
    b.op("dve", lambda h: h.tensor_copy(out=sh1[:], in_=modall[:, 0:8]), reads=[modall], writes=[sh1])
    b.op("dve", lambda h: h.scalar_tensor_tensor(out=gs1[:], in0=modall[:, 8:16], scalar=1.0, in1=lng[:],
                                                 op0=ALU.add, op1=ALU.mult), reads=[modall, lng], writes=[gs1])
    b.op("act", lambda h: h.activation(out=cl[:], in_=sm[:, 6:8], func=AF.Exp, scale=-1.0), reads=[sm], writes=[cl])
    b.op("act", lambda h: h.activation(out=cl[:], in_=cl[:], func=AF.Ln, bias=1.0), reads=[cl], writes=[cl])
    b.op("dve", lambda h: h.tensor_scalar(out=cl[:], in0=cl[:], scalar1=-8.0, scalar2=None, op0=ALU.mult), reads=[cl], writes=[cl])
    b.op("dve", lambda h: h.tensor_tensor(out=lamt[:, 0:64], in0=lamv[:, 0:64], in1=lamv[:, 64:128], op=ALU.mult),
         reads=[lamv], writes=[lamt])
    b.op("dve", lambda h: h.tensor_tensor(out=lamt[:, 64:128], in0=lamv[:, 128:192], in1=lamv[:, 192:256], op=ALU.mult),
         reads=[lamv, lamt], writes=[lamt])
    b.op("dve", lambda h: h.tensor_reduce(out=lams[:, 0:1], in_=lamt[:, 0:64], axis=AX.X, op=ALU.add), reads=[lamt], writes=[lams])
    b.op("dve", lambda h: h.tensor_reduce(out=lams[:, 1:2], in_=lamt[:, 64:128], axis=AX.X, op=ALU.add),
         reads=[lamt, lams], writes=[lams])
    b.op("act", lambda h: h.activation(out=lams[:, 2:4], in_=lams[:, 0:2], func=AF.Exp), reads=[lams], writes=[lams])
    b.op("dve", lambda h: h.scalar_tensor_tensor(out=neglam[:], in0=lams[:, 3:4], scalar=-lambda_init, in1=lams[:, 2:3],
                                                 op0=ALU.add, op1=ALU.subtract), reads=[lams], writes=[neglam])
    b.op("dve", lambda h: h.tensor_scalar(out=gsub[:], in0=sm[:, 8:9], scalar1=(1.0 - lambda_init), scalar2=None,
                                          op0=ALU.mult), reads=[sm], writes=[gsub])

    def rstd_from(ps_ap, dst, inv_n, reads):
        return [
            lambda: b.op("act", lambda h: h.activation(out=dst[:], in_=ps_ap, func=AF.Ln, bias=b.eps[:, 0:1], scale=inv_n),
                         reads=reads + [b.eps], writes=[dst]),
            lambda: b.op("act", lambda h: h.activation(out=dst[:], in_=dst[:], func=AF.Exp, scale=-0.5), reads=[dst], writes=[dst]),
        ]

    def build_front(i):
        L = []
        par = i % 2
        tsl = slice(i * TT, (i + 1) * TT)
        add = lambda en, fn, reads=(), writes=(), inc=True: L.append(
            lambda: b.op(en, fn, reads=list(reads), writes=list(writes), inc=inc))
        L.append(lambda: b.dma("sp", posb[:], pos_d[0:1, tsl].to_broadcast([128, TT]), writes=[posb], sem=s_pos))
        for c2 in range(4):
            for c in (2 * c2, 2 * c2 + 1):
                sq = sqb[c % 2]
                add("act", lambda h, c=c, sq=sq: h.activation(out=sq[:], in_=xt[:, c, :], func=AF.Square), [xt], [sq])
            L.append(None)
            for c in (2 * c2, 2 * c2 + 1):
                sq = sqb[c % 2]
                add("pe", lambda h, c=c, sq=sq: h.matmul(FP[:, :], ones_bf[:, :], sq[:], start=(c == 0), stop=(c == 7)),
                    [sq, ones_bf], [FP])
        L.append(None)
        L.extend(rstd_from(FP[:, :], rstd, 1.0 / D, [FP]))
        for c in range(8):
            t = xn[c % 2]
            add("dve", lambda h, c=c, t=t: h.tensor_tensor(out=t[:], in0=xt[:, c, :], in1=rstd[:], op=ALU.mult), [xt, rstd], [t])
            add("act", lambda h, c=c, t=t: h.activation(out=hT[:, c, :], in_=t[:], func=AF.Identity, scale=gs1[:, c:c + 1],
                                                        bias=sh1[:, c:c + 1]), [t, gs1, sh1], [hT])
        if i + 1 < ntiles:
            L.append(lambda: b.dma("sp", xt[:], xv[:, :, (i + 1) * TT:(i + 2) * TT], writes=[xt], sem=s_x))
        add("dve", lambda h: h.tensor_scalar(out=ang[:], in0=posb[:], scalar1=sm[:, 9:10], scalar2=None, op0=ALU.mult),
            [posb, sm], [ang])
        add("dve", lambda h: h.tensor_scalar(out=kint[:], in0=ang[:], scalar1=1.0 / TWO_PI, scalar2=None, op0=ALU.mult),
            [ang], [kint])
        add("dve", lambda h: h.scalar_tensor_tensor(out=ang[:], in0=kint[:], scalar=-CW1, in1=ang[:], op0=ALU.mult, op1=ALU.add),
            [kint, ang], [ang])
        add("dve", lambda h: h.scalar_tensor_tensor(out=ang[:], in0=kint[:], scalar=-CW2, in1=ang[:], op0=ALU.mult, op1=ALU.add),
            [kint, ang], [ang])
        add("dve", lambda h: h.tensor_scalar(out=ang[:], in0=ang[:], scalar1=-math.pi, scalar2=math.pi, op0=ALU.max, op1=ALU.min),
            [ang], [ang])
        add("dve", lambda h: h.scalar_tensor_tensor(out=absr[:], in0=ang[:], scalar=-1.0, in1=ang[:], op0=ALU.mult, op1=ALU.max),
            [ang], [absr])
        L.append(None)
        L.append(None)
        add("act", lambda h: h.activation(out=sinT[:], in_=ang[:], func=AF.Sin, scale=sgn[:, 0:1]), [ang, sgn], [sinT])
        add("act", lambda h: h.activation(out=cosT[:], in_=absr[:], func=AF.Sin, scale=-1.0, bias=halfpi[:, 0:1]),
            [absr, halfpi], [cosT])

        def proj(oc):
            L.append(None)
            for kc in range(8):
                add("pe", lambda h, kc=kc: h.matmul(FP[:, :], win[:, kc, oc * 128:(oc + 1) * 128], hT[:, kc, :],
                                                    start=(kc == 0), stop=(kc == 7)), [win, hT], [FP], inc=(kc == 7))

        for ch in range(2):
            proj(ch)
            L.append(None)
            add("act", lambda h, ch=ch: h.copy(out=xrb[ch][:, 3:TT + 3], in_=FP[:, :]), [FP], [xrb[ch]])
        for ch in range(2):
            proj(2 + ch)
            L.append(None)
            add("act", lambda h, ch=ch: h.activation(out=gy[ch][:], in_=FP[:, :], func=AF.Gelu_apprx_tanh), [FP], [gy[ch]])
        for which in range(4):
            oc = 4 + 2 * which
            hd = which % 2
            dst = qT[par][hd] if which < 2 else KT[hd][i]
            proj(oc)
            add("dve", lambda h: h.tensor_tensor(out=rt1[:], in0=FP[:, :], in1=cosT[:], op=ALU.mult), [FP, cosT], [rt1])
            proj(oc + 1)
            add("dve", lambda h: h.tensor_tensor(out=rt2[:], in0=FP[:, :], in1=sinT[:], op=ALU.mult), [FP, sinT], [rt2])
            add("pool", lambda h, dst=dst: h.tensor_tensor(out=dst[:], in0=rt1[:], in1=rt2[:], op=ALU.add), [rt1, rt2], [dst])
        for sub in range(4):
            L.append(None)
            for kc in range(8):
                add("pe", lambda h, kc=kc, sub=sub: h.matmul(FP[:, 0:256], hT[:, kc, sub * 128:(sub + 1) * 128],
                                                             win[:, kc, 1536:1792], start=(kc == 0), stop=(kc == 7)),
                    [win, hT], [FP], inc=(kc == 7))
            L.append(None)
            add("act", lambda h, sub=sub: h.copy(out=Vc[i][:, sub, :], in_=FP[:, 0:256]), [FP], [Vc[i]])
        L.append(("A", i))
        for ch in range(2):
            xr = xrb[ch]
            add("dve", lambda h, ch=ch, xr=xr: h.tensor_scalar(out=xc32[:], in0=xr[:, 0:TT], scalar1=convw[:, ch, 0:1],
                                                               scalar2=sm[:, ch:ch + 1], op0=ALU.mult, op1=ALU.add),
                [xr, convw, sm], [xc32])
            for j in range(1, 4):
                add("dve", lambda h, ch=ch, xr=xr, j=j: h.scalar_tensor_tensor(
                    out=xc32[:], in0=xr[:, j:TT + j], scalar=convw[:, ch, j:j + 1], in1=xc32[:], op0=ALU.mult, op1=ALU.add),
                    [xr, convw, xc32], [xc32])
            add("dve", lambda h, xr=xr: h.tensor_copy(out=xr[:, 0:3], in_=xr[:, TT:TT + 3]), [xr], [xr])
            add("pool", lambda h: h.tensor_copy(out=xcb[:], in_=xc32[:]), [xc32], [xcb])
            L.append(None)
            add("pe", lambda h, ch=ch: h.matmul(FP[:, :], gw[:, ch, :], xcb[:, :], start=True, stop=True), [gw, xcb], [FP])
            L.append(None)
            add("act", lambda h, ch=ch: h.activation(out=ra[:], in_=FP[:, :], func=AF.Sigmoid, bias=sm[:, 2 + ch:3 + ch]),
                [FP, sm], [ra])
            L.append(None)
            add("pe", lambda h, ch=ch: h.matmul(FP[:, :], gw[:, 2 + ch, :], xcb[:, :], start=True, stop=True), [gw, xcb], [FP])
            L.append(None)
            add("act", lambda h, ch=ch: h.activation(out=igt[:], in_=FP[:, :], func=AF.Sigmoid, bias=sm[:, 4 + ch:5 + ch]),
                [FP, sm], [igt])
            add("act", lambda h, ch=ch: h.activation(out=ra[:], in_=ra[:], func=AF.Exp, scale=cl[:, ch:ch + 1]), [ra, cl], [ra])
            add("pool", lambda h: h.tensor_tensor(out=a2[:], in0=ra[:], in1=ra[:], op=ALU.mult), [ra], [a2])
            L.append(None)
            add("act", lambda h: h.activation(out=a2[:], in_=a2[:], func=AF.Sqrt, scale=-1.0, bias=1.0), [a2], [a2])
            add("pool", lambda h: h.tensor_tensor(out=igt[:], in0=igt[:], in1=xc32[:], op=ALU.mult), [igt, xc32], [igt])
            add("pool", lambda h: h.tensor_tensor(out=igt[:], in0=igt[:], in1=a2[:], op=ALU.mult), [igt, a2], [igt])
            add("dve", lambda h, ch=ch: h.tensor_tensor_scan(out=hs[:], data0=ra[:], data1=igt[:], initial=hprev[ch][:, 0:1],
                                                             op0=ALU.mult, op1=ALU.add), [ra, igt, hprev[ch]], [hs])
            add("dve", lambda h, ch=ch: h.tensor_copy(out=hprev[ch][:], in_=hs[:, TT - 1:TT]), [hs], [hprev[ch]])
            add("pool", lambda h, ch=ch: h.tensor_tensor(out=mixo[par][:, ch, :], in0=hs[:], in1=gy[ch][:], op=ALU.mult),
                [hs, gy[ch]], [mixo[par]])
        L.append(("B", i))
        return L

    def attention(i, F):
        par = i % 2
        n_iter = 2 * (4 * i + 4)
        nreal = sum(1 for t_ in F if callable(t_))
        per = -(-nreal // (2 * n_iter)) + 1 if n_iter else 0
        ecnt = [0]

        def drain(k):
            n = 0
            while F and n < k:
                t_ = F.popleft()
                if t_ is None:
                    break
                if isinstance(t_, tuple):
                    done.add(t_)
                    continue
                t_()
                n += 1

        for hd in range(2):
            nk = 4 * i + 4
            qh = qT[par][hd]

            def colsl(kt):
                j = kt - 4 * i
                c0 = 128 * j if j > 0 else 0
                return j, c0, slice(c0, TT)

            def qk(kt):
                ti, sub = kt // 4, kt % 4
                j, c0, cs = colsl(kt)
                pss = SB[kt % 2]
                for c in range(2):
                    b.op("pe", lambda h, c=c: h.matmul(
                        pss[c][:, cs], KT[hd][ti][c * 64:(c + 1) * 64, sub * 128:(sub + 1) * 128],
                        qh[c * 64:(c + 1) * 64, cs], start=True, stop=True), reads=[KT[hd][ti], qh], writes=[pss[c]])

            def expo(kt, slot):
                j, c0, cs = colsl(kt)
                pss = SB[kt % 2]
                for c in range(2):
                    e = et[c][slot]
                    b.op("act", lambda h, c=c, e=e: h.activation(out=e[:, cs], in_=pss[c][:, cs], func=AF.Exp, scale=0.125),
                         reads=[pss[c]], writes=[e])
                    if j >= 0:
                        b.op("pool", lambda h, e=e: h.memset(e[64:128, c0:c0 + 64], 0.0), writes=[e])

            def pv(kt, slot):
                ti, sub = kt // 4, kt % 4
                j, c0, cs = colsl(kt)
                for c in range(2):
                    e = et[c][slot]
                    b.op("pe", lambda h, c=c, e=e: h.matmul(
                        po[c][:, cs], Vc[ti][:, sub, hd * 128:(hd + 1) * 128], e[:, cs],
                        start=(kt == 0), stop=(kt == nk - 1)), reads=[Vc[ti], e], writes=[po[c]], inc=False)
                for c in range(2):
                    e = et[c][slot]
                    b.op("pe", lambda h, c=c, e=e: h.matmul(
                        PS[32 * c:32 * c + 32, cs], ones_bf[:, 0:32], e[:, cs], start=(kt == 0), stop=(kt == nk - 1),
                        tile_position=(0, 32 * c)), reads=[ones_bf, e], writes=[PS], inc=(c == 1))

            qk(0)
            for kt in range(nk):
                slot = ecnt[0] % 3
                ecnt[0] += 1
                if kt + 1 < nk:
                    qk(kt + 1)
                expo(kt, slot)
                drain(per)
                pv(kt, slot)
                drain(per)
            b.op("act", lambda h: h.activation(out=rsum[:], in_=PS[0:64, :], func=AF.Ln), reads=[PS], writes=[rsum])
            b.op("act", lambda h: h.activation(out=rsum[:], in_=rsum[:], func=AF.Exp, scale=-1.0), reads=[rsum], writes=[rsum])
            for c in range(2):
                b.op("pe", lambda h, c=c: h.matmul(SB[0][c][:, :], bsel[:, c, :], rsum[:, :], start=True, stop=True),
                     reads=[bsel, rsum], writes=[SB[0][c]])
                b.op("act", lambda h, c=c: h.copy(out=rs[c][:], in_=SB[0][c][:, :]), reads=[SB[0][c]], writes=[rs[c]])
            b.op("dve", lambda h: h.tensor_tensor(out=o0[:], in0=po[0][:, :], in1=rs[0][:], op=ALU.mult),
                 reads=[po[0], rs[0]], writes=[o0])
            b.op("dve", lambda h: h.tensor_tensor(out=o1[:], in0=po[1][:, :], in1=rs[1][:], op=ALU.mult),
                 reads=[po[1], rs[1]], writes=[o1])
            b.op("dve", lambda h: h.scalar_tensor_tensor(out=o0[:], in0=o1[:], scalar=neglam[:, 0:1], in1=o0[:],
                                                         op0=ALU.mult, op1=ALU.add), reads=[o1, neglam, o0], writes=[o0])
            b.op("pool", lambda h: h.tensor_tensor(out=sqo[:], in0=o0[:], in1=o0[:], op=ALU.mult), reads=[o0], writes=[sqo])
            pst = SB[1][0]
            b.op("pe", lambda h: h.matmul(pst[:, :], ones_bf[:, :], sqo[:, :], start=True, stop=True),
                 reads=[ones_bf, sqo], writes=[pst])
            for th in rstd_from(pst[:, :], rs[0], 1.0 / 128, [pst]):
                th()
            b.op("dve", lambda h, hd=hd: h.scalar_tensor_tensor(out=mixo[par][:, 2 + hd, :], in0=o0[:], scalar=gsub[:, 0:1],
                                                                in1=rs[0][:], op0=ALU.mult, op1=ALU.mult),
                 reads=[o0, gsub, rs[0]], writes=[mixo[par]])

    done = set()
    F = collections.deque()

    def drain_until(marker):
        while marker not in done:
            t_ = F.popleft()
            if t_ is None:
                continue
            if isinstance(t_, tuple):
                done.add(t_)
                continue
            t_()

    b.dma("sp", xt[:], xv[:, :, 0:TT], writes=[xt], sem=s_x)
    F.extend(build_front(0))
    drain_until(("A", 0))
    for i in range(ntiles):
        tsl = slice(i * TT, (i + 1) * TT)
        if i + 1 < ntiles:
            F.extend(build_front(i + 1))
        attention(i, F)
        drain_until(("B", i))
        mo = mixo[i % 2]
        g_ = []
        b.dma("sp", mixv[:, rows[0]:rows[0] + 2, tsl], mo[:, 0:2, :], reads=[mo], sem=s_outs[i % 2], group=g_)
        b.dma("sp", mixv[:, rows[2]:rows[2] + 2, tsl], mo[:, 2:4, :], reads=[mo], sem=s_outs[i % 2], group=g_)
        b.seal(g_, s_outs[i % 2])
        if i + 1 < ntiles:
            drain_until(("A", i + 1))
    b.end_phase()


def prep_M(inp, l):
    f32 = np.float32
    inv = (f32(10000.0) ** (-(np.arange(0, 64, 2, dtype=f32)) / f32(64))).astype(f32)
    invf = np.zeros((128, 1), f32)
    sgn = np.zeros((128, 1), f32)
    for p in range(128):
        d = p % 64
        invf[p, 0] = inv[d % 32]
        sgn[p, 0] = -1.0 if d < 32 else 1.0
    swp = np.array([(p // 64) * 64 + ((p % 64) + 32) % 64 for p in range(128)])
    shared = {}
    for hf in range(2):
        ch = hf * 256 + np.arange(256)
        cols = [ch, 512 + ch]
        for base in (1024, 1536):
            for hh in range(2):
                hcol = base + (2 * hf + hh) * 128 + np.arange(128)
                cols.append(hcol)
                cols.append(hcol[swp])
        for hh in range(2):
            cols.append(2048 + (2 * hf + hh) * 128 + np.arange(128))
        cols = np.concatenate(cols)
        win = np.ascontiguousarray(inp["w_in"][l][:, cols])
        convw = np.ascontiguousarray(inp["conv_w"][l][:, ch].T.reshape(2, 128, 4).transpose(1, 0, 2))
        pc = lambda v: v[ch].reshape(2, 128).T
        sm = np.zeros((128, 10), f32)
        sm[:, 0:2] = pc(inp["conv_b"][l])
        sm[:, 2:4] = pc(inp["gate_a_b"][l])
        sm[:, 4:6] = pc(inp["gate_x_b"][l])
        sm[:, 6:8] = pc(inp["lru_lambda"][l])
        sm[:, 8] = inp["subln_g"][l]
        sm[:, 9] = invf[:, 0]
        gw = np.zeros((128, 4, 128), f32)
        for t, key in enumerate(("gate_a_w", "gate_x_w")):
            for c2 in range(2):
                for blk in range(2):
                    n = hf * 4 + c2 * 2 + blk
                    gw[blk * 64:(blk + 1) * 64, t * 2 + c2, blk * 64:(blk + 1) * 64] = inp[key][l][n]
        shared[hf] = dict(win=win, convw=convw, small=sm, gw=gw)
    lamv = np.concatenate([inp["lam_q1"][l], inp["lam_k1"][l], inp["lam_q2"][l], inp["lam_k2"][l]])[None, :].astype(f32)
    adaw = np.ascontiguousarray(inp["ada_w"][l][:, 0:2048])
    adab = np.ascontiguousarray(inp["ada_b"][l][0:2048].reshape(16, 128).T)
    lng = np.ascontiguousarray(inp["ln1_g"][l].reshape(8, 128).T)
    return shared, dict(lamv=lamv, adaw=adaw, adab=adab, lng=lng, sgn=sgn)


STK = 1024
NTOK = S // 2


def emit_F(b, nc, P, tag, l, moe, last, df, xsrc, mixsrc, outv, nsuper, selm=None):
    E = NE if moe else 1
    lng_d, wout_d, modall = df["lng"], df["wout"], df["modall"]
    wg_d, wu_d, wd_d = df["wg"], df["wu"], df["wd"]
    if moe:
        rt_d = df["router"]
    if last:
        fg_d = df["fing"]
    b.begin_phase(tag)
    if True:
        sb = b.sb
        ones_bf = sb("ones_bf", [128, 128], BF16)
        lng = sb("lng", [128, 8], F32)
        g1 = sb("g1", [128, 8], F32)
        sh2 = sb("sh2", [128, 8], F32)
        gs2 = sb("gs2", [128, 8], F32)
        g2 = sb("g2", [128, 8], F32)
        wout = sb("wout", [128, 8, D], BF16)
        x1s = [sb("x1_%d" % i, [128, 8, TT], F32) for i in range(2)]
        h2 = sb("h2", [128, 8, STK], BF16)
        nmix = 1 if selm is not None else 2
        nws = 2 if moe else 3
        mixts = [sb("mixt%d" % i, [128, 8, TT], BF16) for i in range(nmix)]
        sqb = [sb("sqb%d" % i, [128, TT], BF16) for i in range(2)]
        sqf = [sb("sqf%d" % i, [128, TT], F32) for i in range(2)]
        ones_f = sb("ones_f", [128, 128], F32)
        xn = [sb("xn%d" % i, [128, TT], F32) for i in range(2)]
        rstd = sb("rstd", [128, TT], F32)
        wgs = [sb("wgs%d" % i, [128, 8, 512], BF16) for i in range(nws)]
        wus = [sb("wus%d" % i, [128, 8, 512], BF16) for i in range(nws)]
        wds = [sb("wds%d" % i, [128, 4, D], BF16) for i in range(nws)]
        hid = [sb("hid%d" % i, [128, 4, TT], BF16) for i in range(2)]
        sg = [sb("sg%d" % i, [128, TT], F32) for i in range(2)]
        if moe:
            rt = sb("rt", [128, 8, 8], F32)
            h2f = [sb("h2f%d" % i, [128, TT], F32) for i in range(2)]
            gatebc = sb("gatebc", [128, NE, STK], BF16)
            lg = sb("lg", [128, 32], F32)
            G = sb("G", [128, 32], F32)
            msk1 = sb("msk1", [128, 8], F32)
            msk2 = sb("msk2", [128, 8], F32)
            lg2 = sb("lg2", [128, 8], F32)
            mm = sb("mm", [128, 8], F32)
            gT = sb("gT", [8, TT], F32)
            identi = sb("identi", [128, 128], I32)
            ident = sb("ident", [128, 128], F32)
            seli = sb("seli", [8, NE, 128], I32)
            sel = sb("sel", [8, NE, 128], F32)
        if last:
            fg = sb("fg", [128, 8], F32)

        s_small = b.dsem()
        s_wout = b.dsem()
        s_xs = [b.dsem(), b.dsem()]
        s_mixs = [b.dsem() for _ in range(nmix)]
        s_outs = [b.dsem(), b.dsem()]
        w_sems = [[b.dsem() for _ in range(3)] for _ in range(nws)]

        grp = []
        loads = [(lng, lng_d)]
        if selm is not None:
            selsb = sb("selsb", [128, 2], F32)
            loads.append((selsb, selm))
            xalt = sb("xalt", [128, 8, TT], F32)
            malt = sb("malt", [128, 8, TT], BF16)
            s_alt = b.dsem()
        if moe:
            loads.append((rt, rt_d))
        if last:
            loads.append((fg, fg_d))
        for dst, src in loads:
            b.dma("sp", dst[:], src, writes=[dst], sem=s_small, group=grp)
        b.seal(grp, s_small)
        woutv = wout_d.rearrange("(k p) n -> p k n", p=128)
        for kc in range(8):
            b.dma("pool", wout[:, kc, :], woutv[:, kc, :], writes=[wout], sem=s_wout, group=grp)
        b.seal(grp, s_wout)
        b.op("dve", lambda h: h.memset(ones_bf[:], 1.0), writes=[ones_bf])
        b.op("dve", lambda h: h.memset(ones_f[:], 1.0), writes=[ones_f])
        if moe:
            b.op("pool", lambda h: h.iota(identi[:], pattern=[[1, 128]], base=0, channel_multiplier=-1), writes=[identi])
            b.op("dve", lambda h: h.tensor_scalar(out=ident[:], in0=identi[:], scalar1=0.0, scalar2=None, op0=ALU.is_equal),
                 reads=[identi], writes=[ident])
            b.op("pool", lambda h: h.iota(seli[:], pattern=[[-1, NE], [0, 128]], base=0, channel_multiplier=1), writes=[seli])
            b.op("dve", lambda h: h.tensor_scalar(out=sel[:], in0=seli[:], scalar1=0.0, scalar2=None, op0=ALU.is_equal),
                 reads=[seli], writes=[sel])
        b.op("dve", lambda h: h.tensor_copy(out=g1[:], in_=modall[:, 16:24]), reads=[modall], writes=[g1])
        b.op("dve", lambda h: h.tensor_copy(out=sh2[:], in_=modall[:, 24:32]), reads=[modall], writes=[sh2])
        b.op("dve", lambda h: h.scalar_tensor_tensor(out=gs2[:], in0=modall[:, 32:40], scalar=1.0, in1=lng[:],
                                                     op0=ALU.add, op1=ALU.mult), reads=[modall, lng], writes=[gs2])
        b.op("dve", lambda h: h.tensor_copy(out=g2[:], in_=modall[:, 40:48]), reads=[modall], writes=[g2])

        xv = xsrc[0]
        mixv = mixsrc[0]
        pstate = {"n": 0}

        def nps():
            pstate["n"] = (pstate["n"] + 1) % 8
            return P[pstate["n"]]

        blocks = [(e, fb) for e in range(E) for fb in range(DFF // 512)]
        wstate = {"n": 0}

        def load_block(n):
            e, fb = blocks[n % len(blocks)]
            sl = n % nws
            b.dma("pool", wgs[sl][:], wg_d[e].rearrange("(k p) n -> p k n", p=128)[:, :, fb * 512:(fb + 1) * 512],
                  writes=[wgs[sl]], sem=w_sems[sl][0])
            b.dma("pool", wus[sl][:], wu_d[e].rearrange("(k p) n -> p k n", p=128)[:, :, fb * 512:(fb + 1) * 512],
                  writes=[wus[sl]], sem=w_sems[sl][1])
            b.dma("pool", wds[sl][:], wd_d[e][fb * 512:(fb + 1) * 512, :].rearrange("(k p) n -> p k n", p=128),
                  writes=[wds[sl]], sem=w_sems[sl][2])

        total_blocks = nsuper * len(blocks)
        for n0 in range(min(nws - 1, total_blocks)):
            load_block(n0)
        for s_ in range(nsuper):
            if nmix == 2:
                for sub in range(2):
                    t0 = s_ * STK + sub * TT
                    b.dma("sp", mixts[sub][:], mixv[:, :, t0:t0 + TT], writes=[mixts[sub]], sem=s_mixs[sub])
                    b.dma("sp", x1s[sub][:], xv[:, :, t0:t0 + TT], writes=[x1s[sub]], sem=s_xs[sub])
            for sub in range(2):
                t0 = s_ * STK + sub * TT
                csl = slice(sub * TT, (sub + 1) * TT)
                mixt = mixts[sub % nmix]
                x1 = x1s[sub]
                if nmix == 1:
                    b.dma("sp", mixt[:], mixv[:, :, t0:t0 + TT], writes=[mixt], sem=s_mixs[0])
                    b.dma("sp", x1[:], xv[:, :, t0:t0 + TT], writes=[x1], sem=s_xs[sub])
                if selm is not None:
                    g = []
                    b.dma("sp", malt[:], mixsrc[1][:, :, t0:t0 + TT], writes=[malt], sem=s_alt, group=g)
                    b.dma("sp", xalt[:], xsrc[1][:, :, t0:t0 + TT], writes=[xalt], sem=s_alt, group=g)
                    b.seal(g, s_alt)
                    b.op("dve", lambda h: h.tensor_scalar(out=mixt[:], in0=mixt[:], scalar1=selsb[:, 0:1], scalar2=None,
                                                          op0=ALU.mult), reads=[mixt, selsb], writes=[mixt])
                    b.op("dve", lambda h: h.scalar_tensor_tensor(out=mixt[:], in0=malt[:], scalar=selsb[:, 1:2], in1=mixt[:],
                                                                 op0=ALU.mult, op1=ALU.add), reads=[malt, selsb, mixt], writes=[mixt])
                    b.op("dve", lambda h, x1=x1: h.tensor_scalar(out=x1[:], in0=x1[:], scalar1=selsb[:, 0:1],
                                                                 scalar2=None, op0=ALU.mult), reads=[x1, selsb], writes=[x1])
                    b.op("dve", lambda h, x1=x1: h.scalar_tensor_tensor(out=x1[:], in0=xalt[:], scalar=selsb[:, 1:2],
                                                                        in1=x1[:], op0=ALU.mult, op1=ALU.add),
                         reads=[xalt, selsb, x1], writes=[x1])
                for oc in range(8):
                    psb = nps()
                    for kc in range(8):
                        b.op("pe", lambda h, kc=kc, oc=oc, psb=psb, mixt=mixt: h.matmul(
                            psb[:, :], wout[:, kc, oc * 128:(oc + 1) * 128], mixt[:, kc, :], start=(kc == 0), stop=(kc == 7)),
                            reads=[wout, mixt], writes=[psb], inc=(kc == 7))
                    b.op("dve", lambda h, oc=oc, psb=psb, x1=x1: h.scalar_tensor_tensor(
                        out=x1[:, oc, :], in0=psb[:, :], scalar=g1[:, oc:oc + 1], in1=x1[:, oc, :],
                        op0=ALU.mult, op1=ALU.add), reads=[psb, g1, x1], writes=[x1])
                ps_stat = nps()
                for c in range(8):
                    sq = sqf[c % 2]
                    b.op("act", lambda h, c=c, sq=sq, x1=x1: h.activation(out=sq[:], in_=x1[:, c, :], func=AF.Square),
                         reads=[x1], writes=[sq])
                    b.op("pe", lambda h, c=c, sq=sq: h.matmul(ps_stat[:, :], ones_f[:, :], sq[:], start=(c == 0), stop=(c == 7)),
                         reads=[sq, ones_f], writes=[ps_stat], inc=True)
                b.op("act", lambda h: h.activation(out=rstd[:], in_=ps_stat[:, :], func=AF.Sqrt, bias=b.eps[:, 0:1], scale=1.0 / D),
                     reads=[ps_stat], writes=[rstd])
                b.op("dve", lambda h: h.reciprocal(out=rstd[:], in_=rstd[:]), reads=[rstd], writes=[rstd])
                if moe:
                    ps_lg = [nps() for _ in range(4)]
                for c in range(8):
                    t = xn[c % 2]
                    b.op("dve", lambda h, c=c, t=t, x1=x1: h.tensor_tensor(out=t[:], in0=x1[:, c, :], in1=rstd[:], op=ALU.mult),
                         reads=[x1, rstd], writes=[t])
                    b.op("pool", lambda h, c=c, t=t: h.tensor_scalar(
                        out=h2[:, c, csl], in0=t[:], scalar1=gs2[:, c:c + 1], scalar2=sh2[:, c:c + 1],
                        op0=ALU.mult, op1=ALU.add), reads=[t, gs2, sh2], writes=[h2])
                    if moe:
                        hf = h2f[c % 2]
                        b.op("pool", lambda h, c=c, t=t, hf=hf: h.tensor_scalar(
                            out=hf[:], in0=t[:], scalar1=gs2[:, c:c + 1], scalar2=sh2[:, c:c + 1],
                            op0=ALU.mult, op1=ALU.add), reads=[t, gs2, sh2], writes=[hf])
                        for ts_ in range(4):
                            b.op("pe", lambda h, c=c, ts_=ts_, hf=hf: h.matmul(
                                ps_lg[ts_][:, 0:8], hf[:, ts_ * 128:(ts_ + 1) * 128], rt[:, c, :],
                                start=(c == 0), stop=(c == 7)), reads=[hf, rt], writes=[ps_lg[ts_]], inc=(ts_ == 3))
                if moe:
                    for ts_ in range(4):
                        b.op("dve", lambda h, ts_=ts_: h.tensor_copy(out=lg[:, ts_ * 8:(ts_ + 1) * 8], in_=ps_lg[ts_][:, 0:8]),
                             reads=[ps_lg[ts_]], writes=[lg])
                    for ts_ in range(4):
                        lt = slice(ts_ * 8, (ts_ + 1) * 8)
                        b.op("dve", lambda h, lt=lt: h.tensor_reduce(out=mm[:, 0:1], in_=lg[:, lt], axis=AX.X, op=ALU.max),
                             reads=[lg], writes=[mm])
                        b.op("dve", lambda h, lt=lt: h.tensor_scalar(out=msk1[:], in0=lg[:, lt], scalar1=mm[:, 0:1], scalar2=None,
                                                                     op0=ALU.is_equal), reads=[lg, mm], writes=[msk1])
                        b.op("dve", lambda h, lt=lt: h.scalar_tensor_tensor(out=lg2[:], in0=msk1[:], scalar=-1e30, in1=lg[:, lt],
                                                                            op0=ALU.mult, op1=ALU.add),
                             reads=[msk1, lg], writes=[lg2])
                        b.op("dve", lambda h: h.tensor_reduce(out=mm[:, 1:2], in_=lg2[:], axis=AX.X, op=ALU.max),
                             reads=[lg2, mm], writes=[mm])
                        b.op("dve", lambda h: h.tensor_scalar(out=msk2[:], in0=lg2[:], scalar1=mm[:, 1:2], scalar2=None,
                                                              op0=ALU.is_equal), reads=[lg2, mm], writes=[msk2])
                        b.op("dve", lambda h: h.tensor_tensor(out=mm[:, 2:3], in0=mm[:, 1:2], in1=mm[:, 0:1], op=ALU.subtract),
                             reads=[mm], writes=[mm])
                        b.op("act", lambda h: h.activation(out=mm[:, 3:4], in_=mm[:, 2:3], func=AF.Sigmoid, scale=-1.0),
                             reads=[mm], writes=[mm])
                        b.op("act", lambda h: h.activation(out=mm[:, 4:5], in_=mm[:, 2:3], func=AF.Sigmoid),
                             reads=[mm], writes=[mm])
                        b.op("dve", lambda h, lt=lt: h.tensor_scalar(out=G[:, lt], in0=msk1[:], scalar1=mm[:, 3:4], scalar2=None,
                                                                     op0=ALU.mult), reads=[msk1, mm], writes=[G])
                        b.op("dve", lambda h, lt=lt: h.scalar_tensor_tensor(out=G[:, lt], in0=msk2[:], scalar=mm[:, 4:5],
                                                                            in1=G[:, lt], op0=ALU.mult, op1=ALU.add),
                             reads=[msk2, mm, G], writes=[G])
                    ps_T = nps()
                    for ts_ in range(4):
                        b.op("pe", lambda h, ts_=ts_: h.transpose(ps_T[0:8, ts_ * 128:(ts_ + 1) * 128], G[:, ts_ * 8:(ts_ + 1) * 8],
                                                                  ident[:, :]), reads=[G, ident], writes=[ps_T], inc=(ts_ == 3))
                    b.op("act", lambda h: h.copy(out=gT[:], in_=ps_T[0:8, :]), reads=[ps_T], writes=[gT])
                    for e in range(NE):
                        psb = nps()
                        b.op("pe", lambda h, e=e, psb=psb: h.matmul(psb[:, :], sel[:, e, :], gT[:, :], start=True, stop=True),
                             reads=[sel, gT], writes=[psb])
                        b.op("act", lambda h, e=e, psb=psb: h.copy(out=gatebc[:, e, csl], in_=psb[:, :]),
                             reads=[psb], writes=[gatebc])
            for bi in range(len(blocks)):
                n = s_ * len(blocks) + bi
                if n + nws - 1 < total_blocks:
                    load_block(n + nws - 1)
                e, fb = blocks[bi]
                sl = n % nws
                for sub in range(2):
                    csl = slice(sub * TT, (sub + 1) * TT)
                    x1 = x1s[sub]
                    hd_ = hid[sub]
                    for fc in range(4):
                        pg = nps()
                        for kc in range(8):
                            b.op("pe", lambda h, kc=kc, fc=fc, pg=pg: h.matmul(
                                pg[:, :], wgs[sl][:, kc, fc * 128:(fc + 1) * 128], h2[:, kc, csl], start=(kc == 0), stop=(kc == 7)),
                                reads=[wgs[sl], h2], writes=[pg], inc=(kc == 7))
                        pu = nps()
                        for kc in range(8):
                            b.op("pe", lambda h, kc=kc, fc=fc, pu=pu: h.matmul(
                                pu[:, :], wus[sl][:, kc, fc * 128:(fc + 1) * 128], h2[:, kc, csl], start=(kc == 0), stop=(kc == 7)),
                                reads=[wus[sl], h2], writes=[pu], inc=(kc == 7))
                        sgb = sg[fc % 2]
                        b.op("act", lambda h, pg=pg, sgb=sgb: h.activation(out=sgb[:], in_=pg[:, :], func=AF.Silu),
                             reads=[pg], writes=[sgb])
                        if moe:
                            b.op("pool", lambda h, sgb=sgb, e=e: h.tensor_tensor(out=sgb[:], in0=sgb[:], in1=gatebc[:, e, csl],
                                                                                 op=ALU.mult), reads=[sgb, gatebc], writes=[sgb])
                        b.op("dve", lambda h, fc=fc, pu=pu, sgb=sgb, hd_=hd_: h.tensor_tensor(
                            out=hd_[:, fc, :], in0=pu[:, :], in1=sgb[:], op=ALU.mult), reads=[pu, sgb], writes=[hd_])
                    for oc in range(8):
                        pd = nps()
                        for fc in range(4):
                            b.op("pe", lambda h, fc=fc, oc=oc, pd=pd, hd_=hd_: h.matmul(
                                pd[:, :], wds[sl][:, fc, oc * 128:(oc + 1) * 128], hd_[:, fc, :], start=(fc == 0), stop=(fc == 3)),
                                reads=[wds[sl], hd_], writes=[pd], inc=(fc == 3))
                        b.op("dve", lambda h, oc=oc, pd=pd, x1=x1: h.scalar_tensor_tensor(
                            out=x1[:, oc, :], in0=pd[:, :], scalar=g2[:, oc:oc + 1], in1=x1[:, oc, :],
                            op0=ALU.mult, op1=ALU.add), reads=[pd, g2, x1], writes=[x1])
            for sub in range(2):
                x1 = x1s[sub]
                if last:
                    emit_norm(b, nc, x1, slice(0, TT), nps(), ones_bf, sqb, rstd, xn, fg, None, x1, slice(0, TT), TT)
                b.dma("sp", outv[:, :, s_ * STK + sub * TT:s_ * STK + (sub + 1) * TT], x1[:], reads=[x1], sem=s_outs[sub],
                      is_out=last)
    b.end_phase()


def build_fused(ntiles=NT, nsuper_all=S // STK, nsuper_half=NTOK // STK):
    nc = bass.Bass("TRN2", target_bir_lowering=False)
    dr = lambda n, s, dt, k="ExternalInput": nc.dram_tensor(n, list(s), dt, kind=k).ap()
    d = {}
    d["xT"] = dr("xT", [D, S], F32)
    d["pos"] = dr("pos", [1, S], I32)
    d["cvec"] = dr("cvec", [128, 8], F32)
    d["selm"] = dr("selm", [128, 2], F32)
    d["sgn"] = dr("sgn", [128, 1], F32)
    for l in range(DEPTH):
        d["adaw%d" % l] = dr("adaw%d" % l, [D, 6 * D], F32)
        d["adab%d" % l] = dr("adab%d" % l, [128, 48], F32)
        d["lng1_%d" % l] = dr("lng1_%d" % l, [128, 8], F32)
        d["lng2_%d" % l] = dr("lng2_%d" % l, [128, 8], F32)
        d["lamv%d" % l] = dr("lamv%d" % l, [1, 256], F32)
        d["wout%d" % l] = dr("wout%d" % l, [D, D], F32)
        for hh in range(2):
            sfx = "%d_%d" % (l, hh)
            d["win" + sfx] = dr("win" + sfx, [D, 1792], F32)
            d["convw" + sfx] = dr("convw" + sfx, [128, 2, 4], F32)
            d["small" + sfx] = dr("small" + sfx, [128, 10], F32)
            d["gw" + sfx] = dr("gw" + sfx, [128, 4, 128], F32)
    d["wg0"] = dr("wg0", [1, D, DFF], F32)
    d["wu0"] = dr("wu0", [1, D, DFF], F32)
    d["wd0"] = dr("wd0", [1, DFF, D], F32)
    d["router"] = dr("router", [128, 8, 8], F32)
    d["wg1"] = dr("wg1", [NE, D, DFF], F32)
    d["wu1"] = dr("wu1", [NE, D, DFF], F32)
    d["wd1"] = dr("wd1", [NE, DFF, D], F32)
    d["fing"] = dr("fing", [128, 8], F32)
    out_d = dr("xoT", [D, NTOK], F32, "ExternalOutput")
    mixS = [dr("mixS%d" % l, [D, S], BF16, "Internal") for l in range(DEPTH)]
    x1S = dr("x1S", [D, S], F32, "Internal")
    fm = lambda ap: ap.rearrange("(c p) t -> p c t", p=128)

    with contextlib.ExitStack() as es:
        b = B(nc, es)
        P = [b.ps("P%d" % i) for i in range(8)]
        b.eps = b.sb("eps_c", [128, 1], F32)
        b.op("dve", lambda h: h.memset(b.eps[:], EPS), writes=[b.eps])
        b.cs = b.sb("cs_glob", [128, 8], F32)
        modalls = [b.sb("modall%d" % l, [128, 48], F32) for l in range(DEPTH)]
        for l in range(DEPTH):
            emit_mod(b, nc, P, "MOD%d_" % l, d["cvec"], d["adaw%d" % l], d["adab%d" % l], modalls[l], first=(l == 0))
            xv = fm(d["xT"]) if l == 0 else fm(x1S)
            for hh in range(2):
                sfx = "%d_%d" % (l, hh)
                dm = dict(modall=modalls[l], lng=d["lng1_%d" % l],
                          win=d["win" + sfx], convw=d["convw" + sfx], small=d["small" + sfx], sgn=d["sgn"],
                          gw=d["gw" + sfx], lamv=d["lamv%d" % l], pos=d["pos"])
                emit_M(b, nc, P, "M%s_" % sfx, l, dm, xv, fm(mixS[l]), [2 * hh, 2 * hh + 1, 4 + 2 * hh, 5 + 2 * hh],
                       ntiles=ntiles)
            moe = (l % 2 == 1)
            last = (l == DEPTH - 1)
            df = dict(modall=modalls[l], lng=d["lng2_%d" % l],
                      wout=d["wout%d" % l], wg=d["wg%d" % l], wu=d["wu%d" % l], wd=d["wd%d" % l],
                      router=d["router"], fing=d["fing"])
            if not last:
                emit_F(b, nc, P, "F%d_" % l, l, moe, last, df, [xv], [fm(mixS[l])], fm(x1S), nsuper_all)
            else:
                mv = fm(mixS[l])
                emit_F(b, nc, P, "F%d_" % l, l, moe, last, df, [xv[:, :, 0:NTOK], xv[:, :, NTOK:S]],
                       [mv[:, :, 0:NTOK], mv[:, :, NTOK:S]], fm(out_d), nsuper_half, selm=d["selm"])
        b.finish()
    return nc


def make_inputs(inp):
    f32 = np.float32
    per_l = []
    for l in range(DEPTH):
        shared, com = prep_M(inp, l)
        per_l.append((shared, com))
    maps = []
    for core in range(8):
        bb, hf = core // 2, core % 2
        m = dict(xT=np.ascontiguousarray(inp["x"][bb].T),
                 pos=np.ascontiguousarray(inp["positions"][bb][None, :]).astype(np.int32),
                 cvec=np.ascontiguousarray(inp["c"][bb].reshape(8, 128).T),
                 selm=np.tile(np.array([[1.0 - hf, float(hf)]], f32), (128, 1)),
                 sgn=per_l[0][1]["sgn"])
        for l in range(DEPTH):
            shared, com = per_l[l]
            m["adaw%d" % l] = inp["ada_w"][l]
            m["adab%d" % l] = np.ascontiguousarray(inp["ada_b"][l].reshape(48, 128).T)
            m["lng1_%d" % l] = com["lng"]
            m["lng2_%d" % l] = np.ascontiguousarray(inp["ln2_g"][l].reshape(8, 128).T)
            m["lamv%d" % l] = com["lamv"]
            m["wout%d" % l] = inp["w_out"][l]
            for hh in range(2):
                sfx = "%d_%d" % (l, hh)
                for k in ("win", "convw", "small", "gw"):
                    m[k + sfx] = shared[hh][k]
        m["wg0"] = inp["ffn_w_gate"]
        m["wu0"] = inp["ffn_w_up"]
        m["wd0"] = inp["ffn_w_down"]
        m["router"] = np.ascontiguousarray(inp["moe_router"][0].reshape(8, 128, 8).transpose(1, 0, 2))
        m["wg1"] = inp["moe_w_gate"][0]
        m["wu1"] = inp["moe_w_up"][0]
        m["wd1"] = inp["moe_w_down"][0]
        m["fing"] = np.ascontiguousarray(inp["final_g"].reshape(8, 128).T)
        maps.append(m)
    return maps


def kernel(**inp):
    inp = {k: np.asarray(v) for k, v in inp.items()}
    nc = build_fused()
    maps = make_inputs(inp)
    res = run_bass_kernel_spmd(nc, maps, core_ids=list(range(8)))
    out = np.empty((NB, S, D), np.float32)
    for core in range(8):
        bb, hf = core // 2, core % 2
        out[bb, hf * NTOK:(hf + 1) * NTOK, :] = np.asarray(res.results[core]["xoT"]).T
    return out
```

```python
import contextlib
import math
import numpy as np
import ml_dtypes
import concourse.bass as bass
import concourse.mybir as mybir
from concourse.bass_utils import run_bass_kernel_spmd

F32 = mybir.dt.float32
BF16 = mybir.dt.bfloat16
I32 = mybir.dt.int32
AF = mybir.ActivationFunctionType
ALU = mybir.AluOpType
AX = mybir.AxisListType

D = 1024
S = 8192
NB = 4
DEPTH = 2
DFF = 3072
NE = 8
EPS = 1e-6
TT = 512
NT = S // TT
TWO_PI = 2.0 * math.pi
CW1 = 6.28125
CW2 = TWO_PI - CW1


class Tok:
    __slots__ = ("sem", "val", "eng")

    def __init__(self, sem, val, eng):
        self.sem, self.val, self.eng = sem, val, eng


class Buf:
    def __init__(self, t):
        self.t = t
        self.w = None
        self.r = {}

    def __getitem__(self, idx):
        return self.t[idx]


class Eng:
    def __init__(self, name, h, sem):
        self.name, self.h, self.sem = name, h, sem
        self.count = 0
        self.waited = {}
        self.pending = []


class B:
    def __init__(self, nc, es):
        self.nc = nc
        self.es = es
        self.eng = {}
        for name, h in (("pe", nc.tensor), ("act", nc.scalar), ("dve", nc.vector),
                        ("pool", nc.gpsimd), ("sp", nc.sync)):
            sem = es.enter_context(nc.semaphore("s_" + name))
            self.eng[name] = Eng(name, h, sem)
        self.nsem = 0
        self.out_toks = []
        self.psn = 0
        self.dsems = []
        self.pes = None
        self.ptag = ""

    def begin_phase(self, tag):
        self.ptag = tag
        self.pes = contextlib.ExitStack()
        self.pes.__enter__()

    def end_phase(self):
        self.barrier()
        self.pes.__exit__(None, None, None)
        self.pes = None

    def barrier(self):
        toks = [Tok(e.sem, e.count, e) for e in self.eng.values() if e.count > 0]
        toks += [Tok(sm[0], sm[1], None) for sm in self.dsems if sm[1] > 0]
        for e in self.eng.values():
            assert not e.pending
            for t in toks:
                self._wait(e, t)

    def sb(self, name, shape, dt):
        st = self.pes if self.pes is not None else self.es
        return Buf(st.enter_context(self.nc.sbuf_tensor("sb_" + self.ptag + name, list(shape), dt)))

    def ps(self, name):
        return Buf(self.es.enter_context(self.nc.psum_tensor("ps_" + name, [128, 512], F32)))

    def dsem(self):
        self.nsem += 1
        sm = [self.es.enter_context(self.nc.semaphore("d%d" % self.nsem)), 0]
        self.dsems.append(sm)
        return sm

    def _wait(self, e, tok):
        if tok is None:
            return
        if tok.eng is e and e.name in ("pe", "sp"):
            return
        assert tok.val is not None, "unresolved token"
        k = id(tok.sem)
        if e.waited.get(k, 0) >= tok.val:
            return
        e.h.wait_ge(tok.sem, tok.val)
        e.waited[k] = tok.val

    def _deps(self, e, reads, writes):
        for bf in reads:
            self._wait(e, bf.w)
        for bf in writes:
            self._wait(e, bf.w)
            for t in bf.r.values():
                self._wait(e, t)

    def _record(self, tok, reads, writes):
        for bf in writes:
            bf.w = tok
            bf.r = {}
        for bf in reads:
            if bf not in writes:
                bf.r[id(tok.sem)] = tok

    def op(self, en, fn, reads=(), writes=(), inc=True):
        e = self.eng[en]
        self._deps(e, reads, writes)
        ins = fn(e.h)
        tok = Tok(e.sem, None, e)
        if inc:
            e.count += 1
            ins.then_inc(e.sem, 1)
            tok.val = e.count
            for p in e.pending:
                p.val = e.count
            e.pending = []
        else:
            e.pending.append(tok)
        self._record(tok, reads, writes)
        return tok

    def dma(self, qn, out, in_, reads=(), writes=(), sem=None, is_out=False, group=None):
        e = self.eng[qn]
        self._deps(e, reads, writes)
        ins = e.h.dma_start(out=out, in_=in_)
        sem[1] += 16
        ins.then_inc(sem[0], 16)
        tok = Tok(sem[0], sem[1], None)
        self._record(tok, reads, writes)
        if is_out:
            self.out_toks.append(tok)
        if group is not None:
            group.append(tok)
        return tok

    @staticmethod
    def seal(group, sem):
        for t in group:
            t.val = sem[1]
        del group[:]

    def finish(self):
        e = self.eng["sp"]
        last = {}
        for t in self.out_toks:
            last[id(t.sem)] = t
        for t in last.values():
            self._wait(e, t)


def emit_adaln(b, nc, P, adaw_d, ncols, cs_sb, modraw, adaw_slots, adaw_sems):
    nch = ncols // 128
    adv = adaw_d.rearrange("(k p) n -> p k n", p=128)
    psb = P[0]
    for j in range(nch):
        slot = adaw_slots[j % len(adaw_slots)]
        b.dma("sp", slot[:], adv[:, :, j * 128:(j + 1) * 128], writes=[slot], sem=adaw_sems[j % len(adaw_slots)])
        for kc in range(8):
            b.op("pe", lambda h, kc=kc, slot=slot, j=j: h.matmul(
                psb[:, j:j + 1], slot[:, kc, :], cs_sb[:, kc:kc + 1], start=(kc == 0), stop=(kc == 7)),
                reads=[slot, cs_sb], writes=[psb], inc=(kc == 7))
    b.op("dve", lambda h: h.tensor_copy(out=modraw[:, 0:nch], in_=psb[:, 0:nch]), reads=[psb], writes=[modraw])


def emit_mod(b, nc, P, tag, c_d, adaw_d, adab_d, modall, first):
    b.begin_phase(tag)
    sb = b.sb
    adab = sb("adab", [128, 48], F32)
    modraw = sb("modraw", [128, 48], F32)
    slots = [sb("adaw%d" % i, [128, 8, 128], F32) for i in range(4)]
    sems = [b.dsem() for _ in range(4)]
    s_small = b.dsem()
    grp = []
    if first:
        c_sb = sb("c_sb", [128, 8], F32)
        b.dma("sp", c_sb[:], c_d, writes=[c_sb], sem=s_small, group=grp)
    b.dma("sp", adab[:], adab_d, writes=[adab], sem=s_small, group=grp)
    b.seal(grp, s_small)
    if first:
        b.op("act", lambda h: h.activation(out=b.cs[:], in_=c_sb[:], func=AF.Silu), reads=[c_sb], writes=[b.cs])
    emit_adaln(b, nc, P, adaw_d, 6 * D, b.cs, modraw, slots, sems)
    b.op("dve", lambda h: h.tensor_tensor(out=modall[:], in0=modraw[:], in1=adab[:], op=ALU.add),
         reads=[modraw, adab], writes=[modall])
    b.end_phase()


def emit_norm(b, nc, xsrc, xsl, ps_stat, ones_bf, sqb, rstd, xn, gs, sh, hdst, hsl, ncol, gcol0=0):
    for c in range(8):
        sq = sqb[c % 2]
        b.op("act", lambda h, c=c, sq=sq: h.activation(out=sq[:, 0:ncol], in_=xsrc[:, c, xsl], func=AF.Square),
             reads=[xsrc], writes=[sq])
        b.op("pe", lambda h, c=c, sq=sq: h.matmul(ps_stat[:, 0:ncol], ones_bf[:, :], sq[:, 0:ncol],
                                                  start=(c == 0), stop=(c == 7)),
             reads=[sq, ones_bf], writes=[ps_stat], inc=True)
    b.op("act", lambda h: h.activation(out=rstd[:, 0:ncol], in_=ps_stat[:, 0:ncol], func=AF.Sqrt,
                                       bias=b.eps[:, 0:1], scale=1.0 / D), reads=[ps_stat], writes=[rstd])
    b.op("dve", lambda h: h.reciprocal(out=rstd[:, 0:ncol], in_=rstd[:, 0:ncol]), reads=[rstd], writes=[rstd])
    for c in range(8):
        t = xn[c % 2]
        b.op("dve", lambda h, c=c, t=t: h.tensor_tensor(out=t[:, 0:ncol], in0=xsrc[:, c, xsl], in1=rstd[:, 0:ncol],
                                                        op=ALU.mult), reads=[xsrc, rstd], writes=[t])
        if sh is not None:
            b.op("pool", lambda h, c=c, t=t: h.tensor_scalar(
                out=hdst[:, c, hsl], in0=t[:, 0:ncol], scalar1=gs[:, gcol0 + c:gcol0 + c + 1],
                scalar2=sh[:, c:c + 1], op0=ALU.mult, op1=ALU.add), reads=[t, gs, sh], writes=[hdst])
        else:
            b.op("act", lambda h, c=c, t=t: h.activation(
                out=hdst[:, c, hsl], in_=t[:, 0:ncol], func=AF.Identity, scale=gs[:, gcol0 + c:gcol0 + c + 1]),
                reads=[t, gs], writes=[hdst])


def emit_M(b, nc, P, tag, l, dm, xv, mixv, rows, ntiles=NT):
    import collections
    lambda_init = 0.8 - 0.6 * math.exp(-0.3 * l)
    lng_d, modall = dm["lng"], dm["modall"]
    win_d, convw_d, sm_d, sgn_d, gw_d, lamv_d, pos_d = (dm["win"], dm["convw"], dm["small"], dm["sgn"], dm["gw"],
                                                        dm["lamv"], dm["pos"])
    b.begin_phase(tag)
    sb = b.sb
    ones_bf = sb("ones_bf", [128, 128], BF16)
    lng = sb("lng", [128, 8], F32)
    sh1 = sb("sh1", [128, 8], F32)
    gs1 = sb("gs1", [128, 8], F32)
    convw = sb("convw", [128, 2, 4], F32)
    sm = sb("sm", [128, 10], F32)
    nba = sb("nba", [128, 4], F32)
    sgn = sb("sgn", [128, 1], F32)
    halfpi = sb("halfpi", [128, 1], F32)
    gw = sb("gw", [128, 4, 128], BF16)
    lamv = sb("lamv", [128, 256], F32)
    lamt = sb("lamt", [128, 128], F32)
    lams = sb("lams", [128, 4], F32)
    neglam = sb("neglam", [128, 1], F32)
    gsub = sb("gsub", [128, 1], F32)
    cl = sb("cl", [128, 2], F32)
    bsel = sb("bsel", [64, 2, 128], F32)
    win = sb("win", [128, 8, 1792], BF16)
    KT = [[sb("KT%d_%d" % (hd, i), [128, TT], BF16) for i in range(NT)] for hd in range(2)]
    Vc = [sb("V%d" % i, [128, 4, 256], BF16) for i in range(NT)]
    xt = sb("xt", [128, 8, TT], F32)
    sqb = [sb("sqb%d" % i, [128, TT], BF16) for i in range(2)]
    xn = [sb("xn%d" % i, [128, TT], F32) for i in range(2)]
    rstd = sb("rstd", [128, TT], F32)
    hT = sb("hT", [128, 8, TT], BF16)
    xrb = [sb("xrb%d" % i, [128, TT + 3], F32) for i in range(2)]
    gy = [sb("gy%d" % i, [128, TT], F32) for i in range(2)]
    xg = sb("xg", [128, TT], F32)
    gz = sb("gz", [128, TT], F32)
    posb = sb("posb", [128, TT], I32)
    ang = sb("ang", [128, TT], F32)
    kint = sb("kint", [128, TT], I32)
    absr = sb("absr", [128, TT], F32)
    sinT = sb("sinT", [128, TT], F32)
    cosT = sb("cosT", [128, TT], F32)
    rt1 = sb("rt1", [128, TT], F32)
    rt2 = sb("rt2", [128, TT], F32)
    qT = [[sb("qT%d_%d" % (p_, i), [128, TT], BF16) for i in range(2)] for p_ in range(2)]
    xc32 = sb("xc32", [128, TT], F32)
    xcb = sb("xcb", [128, TT], BF16)
    ra = sb("ra", [128, TT], F32)
    a2 = sb("a2", [128, TT], F32)
    igt = sb("igt", [128, TT], F32)
    hs = sb("hs", [128, TT], F32)
    hprev = [sb("hprev%d" % i, [128, 1], F32) for i in range(2)]
    et = [[sb("e%d_%d" % (c, i), [128, TT], BF16) for i in range(3)] for c in range(2)]
    rs = [sb("rs%d" % i, [128, TT], F32) for i in range(2)]
    rsum = sb("rsum", [64, TT], F32)
    o0 = sb("o0", [128, TT], F32)
    o1 = sb("o1", [128, TT], F32)
    sqo = sb("sqo", [128, TT], BF16)
    mixo = [sb("mixo%d" % i, [128, 4, TT], BF16) for i in range(2)]

    s_small = b.dsem()
    s_win = b.dsem()
    s_x = b.dsem()
    s_pos = b.dsem()
    s_outs = [b.dsem(), b.dsem()]
    FP = P[7]
    SB = [[P[0], P[1]], [P[2], P[3]]]
    po = [P[4], P[5]]
    PS = P[6]

    grp = []
    for dst, src in ((lng, lng_d), (convw, convw_d), (sm, sm_d), (sgn, sgn_d)):
        b.dma("sp", dst[:], src, writes=[dst], sem=s_small, group=grp)
    b.dma("sp", lamv[:], lamv_d[0:1, :].to_broadcast([128, 256]), writes=[lamv], sem=s_small, group=grp)
    b.seal(grp, s_small)
    b.dma("pool", gw[:], gw_d, writes=[gw], sem=s_win, group=grp)
    winv = win_d.rearrange("(k p) n -> p k n", p=128)
    for kc in range(8):
        b.dma("pool", win[:, kc, :], winv[:, kc, :], writes=[win], sem=s_win, group=grp)
    b.seal(grp, s_win)
    b.op("dve", lambda h: h.memset(ones_bf[:], 1.0), writes=[ones_bf])
    b.op("dve", lambda h: h.memset(halfpi[:], math.pi / 2), writes=[halfpi])
    b.op("dve", lambda h: h.memset(bsel[:], 0.0), writes=[bsel])
    b.op("dve", lambda h: h.memset(bsel[0:32, 0, :], 1.0 / 32), writes=[bsel])
    b.op("dve", lambda h: h.memset(bsel[32:64, 1, :], 1.0 / 32), writes=[bsel])
    for ch in range(2):
        b.op("dve", lambda h, ch=ch: h.memset(xrb[ch][:, 0:3], 0.0), writes=[xrb[ch]])
        b.op("dve", lambda h, ch=ch: h.memset(hprev[ch][:], 0.0), writes=[hprev[ch]])
    b.op("dve", lambda h: h.tensor_scalar(out=nba[:], in0=sm[:, 2:6], scalar1=-1.0, scalar2=None, op0=ALU.mult),
         reads=[sm], writes=[nba])
    b.op("dve", lambda h: h.tensor_copy(out=sh1[:], in_=modall[:, 0:8]), reads=[modall], writes=[sh1])
    b.op("dve", lambda h: h.scalar_tensor_tensor(out=gs1[:], in0=modall[:, 8:16], scalar=1.0, in1=lng[:],
                                                 op0=ALU.add, op1=ALU.mult), reads=[modall, lng], writes=[gs1])
    b.op("act", lambda h: h.activation(out=cl[:], in_=sm[:, 6:8], func=AF.Exp, scale=-1.0), reads=[sm], writes=[cl])
    b.op("act", lambda h: h.activation(out=cl[:], in_=cl[:], func=AF.Ln, bias=1.0), reads=[cl], writes=[cl])
    b.op("dve", lambda h: h.tensor_scalar(out=cl[:], in0=cl[:], scalar1=-8.0, scalar2=None, op0=ALU.mult), reads=[cl], writes=[cl])
    b.op("dve", lambda h: h.tensor_tensor(out=lamt[:, 0:64], in0=lamv[:, 0:64], in1=lamv[:, 64:128], op=ALU.mult),
         reads=[lamv], writes=[lamt])
    b.op("dve", lambda h: h.tensor_tensor(out=lamt[:, 64:128], in0=lamv[:, 128:192], in1=lamv[:, 192:256], op=ALU.mult),
         reads=[lamv, lamt], writes=[lamt])
    b.op("dve", lambda h: h.tensor_reduce(out=lams[:, 0:1], in_=lamt[:, 0:64], axis=AX.X, op=ALU.add), reads=[lamt], writes=[lams])
    b.op("dve", lambda h: h.tensor_reduce(out=lams[:, 1:2], in_=lamt[:, 64:128], axis=AX.X, op=ALU.add),
         reads=[lamt, lams], writes=[lams])
    b.op("act", lambda h: h.activation(out=lams[:, 2:4], in_=lams[:, 0:2], func=AF.Exp), reads=[lams], writes=[lams])
    b.op("dve", lambda h: h.scalar_tensor_tensor(out=neglam[:], in0=lams[:, 3:4], scalar=-lambda_init, in1=lams[:, 2:3],
                                                 op0=ALU.add, op1=ALU.subtract), reads=[lams], writes=[neglam])
    b.op("dve", lambda h: h.tensor_scalar(out=gsub[:], in0=sm[:, 8:9], scalar1=(1.0 - lambda_init), scalar2=None,
                                          op0=ALU.mult), reads=[sm], writes=[gsub])

    def rstd_from(ps_ap, dst, inv_n, reads):
        return [
            lambda: b.op("act", lambda h: h.activation(out=dst[:], in_=ps_ap, func=AF.Ln, bias=b.eps[:, 0:1], scale=inv_n),
                         reads=reads + [b.eps], writes=[dst]),
            lambda: b.op("act", lambda h: h.activation(out=dst[:], in_=dst[:], func=AF.Exp, scale=-0.5), reads=[dst], writes=[dst]),
        ]

    def build_front(i):
        L = []
        par = i % 2
        tsl = slice(i * TT, (i + 1) * TT)
        add = lambda en, fn, reads=(), writes=(), inc=True: L.append(
            lambda: b.op(en, fn, reads=list(reads), writes=list(writes), inc=inc))
        L.append(lambda: b.dma("sp", posb[:], pos_d[0:1, tsl].to_broadcast([128, TT]), writes=[posb], sem=s_pos))
        for c2 in range(4):
            for c in (2 * c2, 2 * c2 + 1):
                sq = sqb[c % 2]
                add("act", lambda h, c=c, sq=sq: h.activation(out=sq[:], in_=xt[:, c, :], func=AF.Square), [xt], [sq])
            L.append(None)
            for c in (2 * c2, 2 * c2 + 1):
                sq = sqb[c % 2]
                add("pe", lambda h, c=c, sq=sq: h.matmul(FP[:, :], ones_bf[:, :], sq[:], start=(c == 0), stop=(c == 7)),
                    [sq, ones_bf], [FP])
        L.append(None)
        L.extend(rstd_from(FP[:, :], rstd, 1.0 / D, [FP]))
        for c in range(8):
            t = xn[c % 2]
            add("dve", lambda h, c=c, t=t: h.tensor_tensor(out=t[:], in0=xt[:, c, :], in1=rstd[:], op=ALU.mult), [xt, rstd], [t])
            add("act", lambda h, c=c, t=t: h.activation(out=hT[:, c, :], in_=t[:], func=AF.Identity, scale=gs1[:, c:c + 1],
                                                        bias=sh1[:, c:c + 1]), [t, gs1, sh1], [hT])
        if i + 1 < ntiles:
            L.append(lambda: b.dma("sp", xt[:], xv[:, :, (i + 1) * TT:(i + 2) * TT], writes=[xt], sem=s_x))
        add("dve", lambda h: h.tensor_scalar(out=ang[:], in0=posb[:], scalar1=sm[:, 9:10], scalar2=None, op0=ALU.mult),
            [posb, sm], [ang])
        add("dve", lambda h: h.tensor_scalar(out=kint[:], in0=ang[:], scalar1=1.0 / TWO_PI, scalar2=None, op0=ALU.mult),
            [ang], [kint])
        add("dve", lambda h: h.scalar_tensor_tensor(out=ang[:], in0=kint[:], scalar=-CW1, in1=ang[:], op0=ALU.mult, op1=ALU.add),
            [kint, ang], [ang])
        add("dve", lambda h: h.scalar_tensor_tensor(out=ang[:], in0=kint[:], scalar=-CW2, in1=ang[:], op0=ALU.mult, op1=ALU.add),
            [kint, ang], [ang])
        add("dve", lambda h: h.tensor_scalar(out=ang[:], in0=ang[:], scalar1=-math.pi, scalar2=math.pi, op0=ALU.max, op1=ALU.min),
            [ang], [ang])
        add("dve", lambda h: h.scalar_tensor_tensor(out=absr[:], in0=ang[:], scalar=-1.0, in1=ang[:], op0=ALU.mult, op1=ALU.max),
            [ang], [absr])
        L.append(None)
        L.append(None)
        add("act", lambda h: h.activation(out=sinT[:], in_=ang[:], func=AF.Sin, scale=sgn[:, 0:1]), [ang, sgn], [sinT])
        add("act", lambda h: h.activation(out=cosT[:], in_=absr[:], func=AF.Sin, scale=-1.0, bias=halfpi[:, 0:1]),
            [absr, halfpi], [cosT])

        def proj(oc):
            L.append(None)
            for kc in range(8):
                add("pe", lambda h, kc=kc: h.matmul(FP[:, :], win[:, kc, oc * 128:(oc + 1) * 128], hT[:, kc, :],
                                                    start=(kc == 0), stop=(kc == 7)), [win, hT], [FP], inc=(kc == 7))

        for ch in range(2):
            proj(ch)
            L.append(None)
            add("act", lambda h, ch=ch: h.copy(out=xrb[ch][:, 3:TT + 3], in_=FP[:, :]), [FP], [xrb[ch]])
        for ch in range(2):
            proj(2 + ch)
            L.append(None)
            add("act", lambda h, ch=ch: h.activation(out=gy[ch][:], in_=FP[:, :], func=AF.Gelu_apprx_tanh), [FP], [gy[ch]])
        for which in range(4):
            oc = 4 + 2 * which
            hd = which % 2
            dst = qT[par][hd] if which < 2 else KT[hd][i]
            proj(oc)
            add("dve", lambda h: h.tensor_tensor(out=rt1[:], in0=FP[:, :], in1=cosT[:], op=ALU.mult), [FP, cosT], [rt1])
            proj(oc + 1)
            add("dve", lambda h: h.tensor_tensor(out=rt2[:], in0=FP[:, :], in1=sinT[:], op=ALU.mult), [FP, sinT], [rt2])
            add("pool", lambda h, dst=dst: h.tensor_tensor(out=dst[:], in0=rt1[:], in1=rt2[:], op=ALU.add), [rt1, rt2], [dst])
        for sub in range(4):
            L.append(None)
            for kc in range(8):
                add("pe", lambda h, kc=kc, sub=sub: h.matmul(FP[:, 0:256], hT[:, kc, sub * 128:(sub + 1) * 128],
                                                             win[:, kc, 1536:1792], start=(kc == 0), stop=(kc == 7)),
                    [win, hT], [FP], inc=(kc == 7))
            L.append(None)
            add("act", lambda h, sub=sub: h.copy(out=Vc[i][:, sub, :], in_=FP[:, 0:256]), [FP], [Vc[i]])
        L.append(("A", i))
        for ch in range(2):
            xr = xrb[ch]
            add("dve", lambda h, ch=ch, xr=xr: h.tensor_scalar(out=xc32[:], in0=xr[:, 0:TT], scalar1=convw[:, ch, 0:1],
                                                               scalar2=sm[:, ch:ch + 1], op0=ALU.mult, op1=ALU.add),
                [xr, convw, sm], [xc32])
            for j in range(1, 4):
                add("dve", lambda h, ch=ch, xr=xr, j=j: h.scalar_tensor_tensor(
                    out=xc32[:], in0=xr[:, j:TT + j], scalar=convw[:, ch, j:j + 1], in1=xc32[:], op0=ALU.mult, op1=ALU.add),
                    [xr, convw, xc32], [xc32])
            add("dve", lambda h, xr=xr: h.tensor_copy(out=xr[:, 0:3], in_=xr[:, TT:TT + 3]), [xr], [xr])
            add("pool", lambda h: h.tensor_copy(out=xcb[:], in_=xc32[:]), [xc32], [xcb])
            L.append(None)
            add("pe", lambda h, ch=ch: h.matmul(FP[:, :], gw[:, ch, :], xcb[:, :], start=True, stop=True), [gw, xcb], [FP])
            L.append(None)
            add("act", lambda h, ch=ch: h.activation(out=ra[:], in_=FP[:, :], func=AF.Sigmoid, bias=sm[:, 2 + ch:3 + ch]),
                [FP, sm], [ra])
            L.append(None)
            add("pe", lambda h, ch=ch: h.matmul(FP[:, :], gw[:, 2 + ch, :], xcb[:, :], start=True, stop=True), [gw, xcb], [FP])
            L.append(None)
            add("act", lambda h, ch=ch: h.activation(out=igt[:], in_=FP[:, :], func=AF.Sigmoid, bias=sm[:, 4 + ch:5 + ch]),
                [FP, sm], [igt])
            add("act", lambda h, ch=ch: h.activation(out=ra[:], in_=ra[:], func=AF.Exp, scale=cl[:, ch:ch + 1]), [ra, cl], [ra])
            add("pool", lambda h: h.tensor_tensor(out=a2[:], in0=ra[:], in1=ra[:], op=ALU.mult), [ra], [a2])
            L.append(None)
            add("act", lambda h: h.activation(out=a2[:], in_=a2[:], func=AF.Sqrt, scale=-1.0, bias=1.0), [a2], [a2])
            add("pool", lambda h: h.tensor_tensor(out=igt[:], in0=igt[:], in1=xc32[:], op=ALU.mult), [igt, xc32], [igt])
            add("pool", lambda h: h.tensor_tensor(out=igt[:], in0=igt[:], in1=a2[:], op=ALU.mult), [igt, a2], [igt])
            add("dve", lambda h, ch=ch: h.tensor_tensor_scan(out=hs[:], data0=ra[:], data1=igt[:], initial=hprev[ch][:, 0:1],
                                                             op0=ALU.mult, op1=ALU.add), [ra, igt, hprev[ch]], [hs])
            add("dve", lambda h, ch=ch: h.tensor_copy(out=hprev[ch][:], in_=hs[:, TT - 1:TT]), [hs], [hprev[ch]])
            add("pool", lambda h, ch=ch: h.tensor_tensor(out=mixo[par][:, ch, :], in0=hs[:], in1=gy[ch][:], op=ALU.mult),
                [hs, gy[ch]], [mixo[par]])
        L.append(("B", i))
        return L

    def attention(i, F):
        par = i % 2
        n_iter = 2 * (4 * i + 4)
        nreal = sum(1 for t_ in F if callable(t_))
        per = -(-nreal // (2 * n_iter)) + 1 if n_iter else 0
        ecnt = [0]

        def drain(k):
            n = 0
            while F and n < k:
                t_ = F.popleft()
                if t_ is None:
                    break
                if isinstance(t_, tuple):
                    done.add(t_)
                    continue
                t_()
                n += 1

        for hd in range(2):
            nk = 4 * i + 4
            qh = qT[par][hd]

            def colsl(kt):
                j = kt - 4 * i
                c0 = 128 * j if j > 0 else 0
                return j, c0, slice(c0, TT)

            def qk(kt):
                ti, sub = kt // 4, kt % 4
                j, c0, cs = colsl(kt)
                pss = SB[kt % 2]
                for c in range(2):
                    b.op("pe", lambda h, c=c: h.matmul(
                        pss[c][:, cs], KT[hd][ti][c * 64:(c + 1) * 64, sub * 128:(sub + 1) * 128],
                        qh[c * 64:(c + 1) * 64, cs], start=True, stop=True), reads=[KT[hd][ti], qh], writes=[pss[c]])

            def expo(kt, slot):
                j, c0, cs = colsl(kt)
                pss = SB[kt % 2]
                for c in range(2):
                    e = et[c][slot]
                    b.op("act", lambda h, c=c, e=e: h.activation(out=e[:, cs], in_=pss[c][:, cs], func=AF.Exp, scale=0.125),
                         reads=[pss[c]], writes=[e])
                    if j >= 0:
                        b.op("pool", lambda h, e=e: h.memset(e[64:128, c0:c0 + 64], 0.0), writes=[e])

            def pv(kt, slot):
                ti, sub = kt // 4, kt % 4
                j, c0, cs = colsl(kt)
                for c in range(2):
                    e = et[c][slot]
                    b.op("pe", lambda h, c=c, e=e: h.matmul(
                        po[c][:, cs], Vc[ti][:, sub, hd * 128:(hd + 1) * 128], e[:, cs],
                        start=(kt == 0), stop=(kt == nk - 1)), reads=[Vc[ti], e], writes=[po[c]], inc=False)
                for c in range(2):
                    e = et[c][slot]
                    b.op("pe", lambda h, c=c, e=e: h.matmul(
                        PS[32 * c:32 * c + 32, cs], ones_bf[:, 0:32], e[:, cs], start=(kt == 0), stop=(kt == nk - 1),
                        tile_position=(0, 32 * c)), reads=[ones_bf, e], writes=[PS], inc=(c == 1))

            qk(0)
            for kt in range(nk):
                slot = ecnt[0] % 3
                ecnt[0] += 1
                if kt + 1 < nk:
                    qk(kt + 1)
                expo(kt, slot)
                drain(per)
                pv(kt, slot)
                drain(per)
            b.op("act", lambda h: h.activation(out=rsum[:], in_=PS[0:64, :], func=AF.Ln), reads=[PS], writes=[rsum])
            b.op("act", lambda h: h.activation(out=rsum[:], in_=rsum[:], func=AF.Exp, scale=-1.0), reads=[rsum], writes=[rsum])
            for c in range(2):
                b.op("pe", lambda h, c=c: h.matmul(SB[0][c][:, :], bsel[:, c, :], rsum[:, :], start=True, stop=True),
                     reads=[bsel, rsum], writes=[SB[0][c]])
                b.op("act", lambda h, c=c: h.copy(out=rs[c][:], in_=SB[0][c][:, :]), reads=[SB[0][c]], writes=[rs[c]])
            b.op("dve", lambda h: h.tensor_tensor(out=o0[:], in0=po[0][:, :], in1=rs[0][:], op=ALU.mult),
                 reads=[po[0], rs[0]], writes=[o0])
            b.op("dve", lambda h: h.tensor_tensor(out=o1[:], in0=po[1][:, :], in1=rs[1][:], op=ALU.mult),
                 reads=[po[1], rs[1]], writes=[o1])
            b.op("dve", lambda h: h.scalar_tensor_tensor(out=o0[:], in0=o1[:], scalar=neglam[:, 0:1], in1=o0[:],
                                                         op0=ALU.mult, op1=ALU.add), reads=[o1, neglam, o0], writes=[o0])
            b.op("pool", lambda h: h.tensor_tensor(out=sqo[:], in0=o0[:], in1=o0[:], op=ALU.mult), reads=[o0], writes=[sqo])
            pst = SB[1][0]
            b.op("pe", lambda h: h.matmul(pst[:, :], ones_bf[:, :], sqo[:, :], start=True, stop=True),
                 reads=[ones_bf, sqo], writes=[pst])
            for th in rstd_from(pst[:, :], rs[0], 1.0 / 128, [pst]):
                th()
            b.op("dve", lambda h, hd=hd: h.scalar_tensor_tensor(out=mixo[par][:, 2 + hd, :], in0=o0[:], scalar=gsub[:, 0:1],
                                                                in1=rs[0][:], op0=ALU.mult, op1=ALU.mult),
                 reads=[o0, gsub, rs[0]], writes=[mixo[par]])

    done = set()
    F = collections.deque()

    def drain_until(marker):
        while marker not in done:
            t_ = F.popleft()
            if t_ is None:
                continue
            if isinstance(t_, tuple):
                done.add(t_)
                continue
            t_()

    b.dma("sp", xt[:], xv[:, :, 0:TT], writes=[xt], sem=s_x)
    F.extend(build_front(0))
    drain_until(("A", 0))
    for i in range(ntiles):
        tsl = slice(i * TT, (i + 1) * TT)
        if i + 1 < ntiles:
            F.extend(build_front(i + 1))
        attention(i, F)
        drain_until(("B", i))
        mo = mixo[i % 2]
        g_ = []
        b.dma("sp", mixv[:, rows[0]:rows[0] + 2, tsl], mo[:, 0:2, :], reads=[mo], sem=s_outs[i % 2], group=g_)
        b.dma("sp", mixv[:, rows[2]:rows[2] + 2, tsl], mo[:, 2:4, :], reads=[mo], sem=s_outs[i % 2], group=g_)
        b.seal(g_, s_outs[i % 2])
        if i + 1 < ntiles:
            drain_until(("A", i + 1))
    b.end_phase()


def prep_M(inp, l):
    f32 = np.float32
    inv = (f32(10000.0) ** (-(np.arange(0, 64, 2, dtype=f32)) / f32(64))).astype(f32)
    invf = np.zeros((128, 1), f32)
    sgn = np.zeros((128, 1), f32)
    for p in range(128):
        d = p % 64
        invf[p, 0] = inv[d % 32]
        sgn[p, 0] = -1.0 if d < 32 else 1.0
    swp = np.array([(p // 64) * 64 + ((p % 64) + 32) % 64 for p in range(128)])
    shared = {}
    for hf in range(2):
        ch = hf * 256 + np.arange(256)
        cols = [ch, 512 + ch]
        for base in (1024, 1536):
            for hh in range(2):
                hcol = base + (2 * hf + hh) * 128 + np.arange(128)
                cols.append(hcol)
                cols.append(hcol[swp])
        for hh in range(2):
            cols.append(2048 + (2 * hf + hh) * 128 + np.arange(128))
        cols = np.concatenate(cols)
        win = np.ascontiguousarray(inp["w_in"][l][:, cols])
        convw = np.ascontiguousarray(inp["conv_w"][l][:, ch].T.reshape(2, 128, 4).transpose(1, 0, 2))
        pc = lambda v: v[ch].reshape(2, 128).T
        sm = np.zeros((128, 10), f32)
        sm[:, 0:2] = pc(inp["conv_b"][l])
        sm[:, 2:4] = pc(inp["gate_a_b"][l])
        sm[:, 4:6] = pc(inp["gate_x_b"][l])
        sm[:, 6:8] = pc(inp["lru_lambda"][l])
        sm[:, 8] = inp["subln_g"][l]
        sm[:, 9] = invf[:, 0]
        gw = np.zeros((128, 4, 128), f32)
        for t, key in enumerate(("gate_a_w", "gate_x_w")):
            for c2 in range(2):
                for blk in range(2):
                    n = hf * 4 + c2 * 2 + blk
                    gw[blk * 64:(blk + 1) * 64, t * 2 + c2, blk * 64:(blk + 1) * 64] = inp[key][l][n]
        shared[hf] = dict(win=win, convw=convw, small=sm, gw=gw)
    lamv = np.concatenate([inp["lam_q1"][l], inp["lam_k1"][l], inp["lam_q2"][l], inp["lam_k2"][l]])[None, :].astype(f32)
    adaw = np.ascontiguousarray(inp["ada_w"][l][:, 0:2048])
    adab = np.ascontiguousarray(inp["ada_b"][l][0:2048].reshape(16, 128).T)
    lng = np.ascontiguousarray(inp["ln1_g"][l].reshape(8, 128).T)
    return shared, dict(lamv=lamv, adaw=adaw, adab=adab, lng=lng, sgn=sgn)


STK = 1024
NTOK = S // 2


def emit_F(b, nc, P, tag, l, moe, last, df, xsrc, mixsrc, outv, nsuper, selm=None):
    E = NE if moe else 1
    lng_d, wout_d, modall = df["lng"], df["wout"], df["modall"]
    wg_d, wu_d, wd_d = df["wg"], df["wu"], df["wd"]
    if moe:
        rt_d = df["router"]
    if last:
        fg_d = df["fing"]
    b.begin_phase(tag)
    if True:
        sb = b.sb
        ones_bf = sb("ones_bf", [128, 128], BF16)
        lng = sb("lng", [128, 8], F32)
        g1 = sb("g1", [128, 8], F32)
        sh2 = sb("sh2", [128, 8], F32)
        gs2 = sb("gs2", [128, 8], F32)
        g2 = sb("g2", [128, 8], F32)
        wout = sb("wout", [128, 8, D], BF16)
        x1s = [sb("x1_%d" % i, [128, 8, TT], F32) for i in range(2)]
        h2 = sb("h2", [128, 8, STK], BF16)
        nmix = 1 if selm is not None else 2
        nws = 2 if moe else 3
        mixts = [sb("mixt%d" % i, [128, 8, TT], BF16) for i in range(nmix)]
        sqb = [sb("sqb%d" % i, [128, TT], BF16) for i in range(2)]
        sqf = [sb("sqf%d" % i, [128, TT], F32) for i in range(2)]
        ones_f = sb("ones_f", [128, 128], F32)
        xn = [sb("xn%d" % i, [128, TT], F32) for i in range(2)]
        rstd = sb("rstd", [128, TT], F32)
        wgs = [sb("wgs%d" % i, [128, 8, 512], BF16) for i in range(nws)]
        wus = [sb("wus%d" % i, [128, 8, 512], BF16) for i in range(nws)]
        wds = [sb("wds%d" % i, [128, 4, D], BF16) for i in range(nws)]
        hid = [sb("hid%d" % i, [128, 4, TT], BF16) for i in range(2)]
        sg = [sb("sg%d" % i, [128, TT], F32) for i in range(2)]
        if moe:
            rt = sb("rt", [128, 8, 8], F32)
            h2f = [sb("h2f%d" % i, [128, TT], F32) for i in range(2)]
            gatebc = sb("gatebc", [128, NE, STK], BF16)
            lg = sb("lg", [128, 32], F32)
            G = sb("G", [128, 32], F32)
            msk1 = sb("msk1", [128, 8], F32)
            msk2 = sb("msk2", [128, 8], F32)
            lg2 = sb("lg2", [128, 8], F32)
            mm = sb("mm", [128, 8], F32)
            gT = sb("gT", [8, TT], F32)
            identi = sb("identi", [128, 128], I32)
            ident = sb("ident", [128, 128], F32)
            seli = sb("seli", [8, NE, 128], I32)
            sel = sb("sel", [8, NE, 128], F32)
        if last:
            fg = sb("fg", [128, 8], F32)

        s_small = b.dsem()
        s_wout = b.dsem()
        s_xs = [b.dsem(), b.dsem()]
        s_mixs = [b.dsem() for _ in range(nmix)]
        s_outs = [b.dsem(), b.dsem()]
        w_sems = [[b.dsem() for _ in range(3)] for _ in range(nws)]

        grp = []
        loads = [(lng, lng_d)]
        if selm is not None:
            selsb = sb("selsb", [128, 2], F32)
            loads.append((selsb, selm))
            xalt = sb("xalt", [128, 8, TT], F32)
            malt = sb("malt", [128, 8, TT], BF16)
            s_alt = b.dsem()
        if moe:
            loads.append((rt, rt_d))
        if last:
            loads.append((fg, fg_d))
        for dst, src in loads:
            b.dma("sp", dst[:], src, writes=[dst], sem=s_small, group=grp)
        b.seal(grp, s_small)
        woutv = wout_d.rearrange("(k p) n -> p k n", p=128)
        for kc in range(8):
            b.dma("pool", wout[:, kc, :], woutv[:, kc, :], writes=[wout], sem=s_wout, group=grp)
        b.seal(grp, s_wout)
        b.op("dve", lambda h: h.memset(ones_bf[:], 1.0), writes=[ones_bf])
        b.op("dve", lambda h: h.memset(ones_f[:], 1.0), writes=[ones_f])
        if moe:
            b.op("pool", lambda h: h.iota(identi[:], pattern=[[1, 128]], base=0, channel_multiplier=-1), writes=[identi])
            b.op("dve", lambda h: h.tensor_scalar(out=ident[:], in0=identi[:], scalar1=0.0, scalar2=None, op0=ALU.is_equal),
                 reads=[identi], writes=[ident])
            b.op("pool", lambda h: h.iota(seli[:], pattern=[[-1, NE], [0, 128]], base=0, channel_multiplier=1), writes=[seli])
            b.op("dve", lambda h: h.tensor_scalar(out=sel[:], in0=seli[:], scalar1=0.0, scalar2=None, op0=ALU.is_equal),
                 reads=[seli], writes=[sel])
        b.op("dve", lambda h: h.tensor_copy(out=g1[:], in_=modall[:, 16:24]), reads=[modall], writes=[g1])
        b.op("dve", lambda h: h.tensor_copy(out=sh2[:], in_=modall[:, 24:32]), reads=[modall], writes=[sh2])
        b.op("dve", lambda h: h.scalar_tensor_tensor(out=gs2[:], in0=modall[:, 32:40], scalar=1.0, in1=lng[:],
                                                     op0=ALU.add, op1=ALU.mult), reads=[modall, lng], writes=[gs2])
        b.op("dve", lambda h: h.tensor_copy(out=g2[:], in_=modall[:, 40:48]), reads=[modall], writes=[g2])

        xv = xsrc[0]
        mixv = mixsrc[0]
        pstate = {"n": 0}

        def nps():
            pstate["n"] = (pstate["n"] + 1) % 8
            return P[pstate["n"]]

        blocks = [(e, fb) for e in range(E) for fb in range(DFF // 512)]
        wstate = {"n": 0}

        def load_block(n):
            e, fb = blocks[n % len(blocks)]
            sl = n % nws
            b.dma("pool", wgs[sl][:], wg_d[e].rearrange("(k p) n -> p k n", p=128)[:, :, fb * 512:(fb + 1) * 512],
                  writes=[wgs[sl]], sem=w_sems[sl][0])
            b.dma("pool", wus[sl][:], wu_d[e].rearrange("(k p) n -> p k n", p=128)[:, :, fb * 512:(fb + 1) * 512],
                  writes=[wus[sl]], sem=w_sems[sl][1])
            b.dma("pool", wds[sl][:], wd_d[e][fb * 512:(fb + 1) * 512, :].rearrange("(k p) n -> p k n", p=128),
                  writes=[wds[sl]], sem=w_sems[sl][2])

        total_blocks = nsuper * len(blocks)
        for n0 in range(min(nws - 1, total_blocks)):
            load_block(n0)
        for s_ in range(nsuper):
            if nmix == 2:
                for sub in range(2):
                    t0 = s_ * STK + sub * TT
                    b.dma("sp", mixts[sub][:], mixv[:, :, t0:t0 + TT], writes=[mixts[sub]], sem=s_mixs[sub])
                    b.dma("sp", x1s[sub][:], xv[:, :, t0:t0 + TT], writes=[x1s[sub]], sem=s_xs[sub])
            for sub in range(2):
                t0 = s_ * STK + sub * TT
                csl = slice(sub * TT, (sub + 1) * TT)
                mixt = mixts[sub % nmix]
                x1 = x1s[sub]
                if nmix == 1:
                    b.dma("sp", mixt[:], mixv[:, :, t0:t0 + TT], writes=[mixt], sem=s_mixs[0])
                    b.dma("sp", x1[:], xv[:, :, t0:t0 + TT], writes=[x1], sem=s_xs[sub])
                if selm is not None:
                    g = []
                    b.dma("sp", malt[:], mixsrc[1][:, :, t0:t0 + TT], writes=[malt], sem=s_alt, group=g)
                    b.dma("sp", xalt[:], xsrc[1][:, :, t0:t0 + TT], writes=[xalt], sem=s_alt, group=g)
                    b.seal(g, s_alt)
                    b.op("dve", lambda h: h.tensor_scalar(out=mixt[:], in0=mixt[:], scalar1=selsb[:, 0:1], scalar2=None,
                                                          op0=ALU.mult), reads=[mixt, selsb], writes=[mixt])
                    b.op("dve", lambda h: h.scalar_tensor_tensor(out=mixt[:], in0=malt[:], scalar=selsb[:, 1:2], in1=mixt[:],
                                                                 op0=ALU.mult, op1=ALU.add), reads=[malt, selsb, mixt], writes=[mixt])
                    b.op("dve", lambda h, x1=x1: h.tensor_scalar(out=x1[:], in0=x1[:], scalar1=selsb[:, 0:1],
                                                                 scalar2=None, op0=ALU.mult), reads=[x1, selsb], writes=[x1])
                    b.op("dve", lambda h, x1=x1: h.scalar_tensor_tensor(out=x1[:], in0=xalt[:], scalar=selsb[:, 1:2],
                                                                        in1=x1[:], op0=ALU.mult, op1=ALU.add),
                         reads=[xalt, selsb, x1], writes=[x1])
                for oc in range(8):
                    psb = nps()
                    for kc in range(8):
                        b.op("pe", lambda h, kc=kc, oc=oc, psb=psb, mixt=mixt: h.matmul(
                            psb[:, :], wout[:, kc, oc * 128:(oc + 1) * 128], mixt[:, kc, :], start=(kc == 0), stop=(kc == 7)),
                            reads=[wout, mixt], writes=[psb], inc=(kc == 7))
                    b.op("dve", lambda h, oc=oc, psb=psb, x1=x1: h.scalar_tensor_tensor(
                        out=x1[:, oc, :], in0=psb[:, :], scalar=g1[:, oc:oc + 1], in1=x1[:, oc, :],
                        op0=ALU.mult, op1=ALU.add), reads=[psb, g1, x1], writes=[x1])
                ps_stat = nps()
                for c in range(8):
                    sq = sqf[c % 2]
                    b.op("act", lambda h, c=c, sq=sq, x1=x1: h.activation(out=sq[:], in_=x1[:, c, :], func=AF.Square),
                         reads=[x1], writes=[sq])
                    b.op("pe", lambda h, c=c, sq=sq: h.matmul(ps_stat[:, :], ones_f[:, :], sq[:], start=(c == 0), stop=(c == 7)),
                         reads=[sq, ones_f], writes=[ps_stat], inc=True)
                b.op("act", lambda h: h.activation(out=rstd[:], in_=ps_stat[:, :], func=AF.Sqrt, bias=b.eps[:, 0:1], scale=1.0 / D),
                     reads=[ps_stat], writes=[rstd])
                b.op("dve", lambda h: h.reciprocal(out=rstd[:], in_=rstd[:]), reads=[rstd], writes=[rstd])
                if moe:
                    ps_lg = [nps() for _ in range(4)]
                for c in range(8):
                    t = xn[c % 2]
                    b.op("dve", lambda h, c=c, t=t, x1=x1: h.tensor_tensor(out=t[:], in0=x1[:, c, :], in1=rstd[:], op=ALU.mult),
                         reads=[x1, rstd], writes=[t])
                    b.op("pool", lambda h, c=c, t=t: h.tensor_scalar(
                        out=h2[:, c, csl], in0=t[:], scalar1=gs2[:, c:c + 1], scalar2=sh2[:, c:c + 1],
                        op0=ALU.mult, op1=ALU.add), reads=[t, gs2, sh2], writes=[h2])
                    if moe:
                        hf = h2f[c % 2]
                        b.op("pool", lambda h, c=c, t=t, hf=hf: h.tensor_scalar(
                            out=hf[:], in0=t[:], scalar1=gs2[:, c:c + 1], scalar2=sh2[:, c:c + 1],
                            op0=ALU.mult, op1=ALU.add), reads=[t, gs2, sh2], writes=[hf])
                        for ts_ in range(4):
                            b.op("pe", lambda h, c=c, ts_=ts_, hf=hf: h.matmul(
                                ps_lg[ts_][:, 0:8], hf[:, ts_ * 128:(ts_ + 1) * 128], rt[:, c, :],
                                start=(c == 0), stop=(c == 7)), reads=[hf, rt], writes=[ps_lg[ts_]], inc=(ts_ == 3))
                if moe:
                    for ts_ in range(4):
                        b.op("dve", lambda h, ts_=ts_: h.tensor_copy(out=lg[:, ts_ * 8:(ts_ + 1) * 8], in_=ps_lg[ts_][:, 0:8]),
                             reads=[ps_lg[ts_]], writes=[lg])
                    for ts_ in range(4):
                        lt = slice(ts_ * 8, (ts_ + 1) * 8)
                        b.op("dve", lambda h, lt=lt: h.tensor_reduce(out=mm[:, 0:1], in_=lg[:, lt], axis=AX.X, op=ALU.max),
                             reads=[lg], writes=[mm])
                        b.op("dve", lambda h, lt=lt: h.tensor_scalar(out=msk1[:], in0=lg[:, lt], scalar1=mm[:, 0:1], scalar2=None,
                                                                     op0=ALU.is_equal), reads=[lg, mm], writes=[msk1])
                        b.op("dve", lambda h, lt=lt: h.scalar_tensor_tensor(out=lg2[:], in0=msk1[:], scalar=-1e30, in1=lg[:, lt],
                                                                            op0=ALU.mult, op1=ALU.add),
                             reads=[msk1, lg], writes=[lg2])
                        b.op("dve", lambda h: h.tensor_reduce(out=mm[:, 1:2], in_=lg2[:], axis=AX.X, op=ALU.max),
                             reads=[lg2, mm], writes=[mm])
                        b.op("dve", lambda h: h.tensor_scalar(out=msk2[:], in0=lg2[:], scalar1=mm[:, 1:2], scalar2=None,
                                                              op0=ALU.is_equal), reads=[lg2, mm], writes=[msk2])
                        b.op("dve", lambda h: h.tensor_tensor(out=mm[:, 2:3], in0=mm[:, 1:2], in1=mm[:, 0:1], op=ALU.subtract),
                             reads=[mm], writes=[mm])
                        b.op("act", lambda h: h.activation(out=mm[:, 3:4], in_=mm[:, 2:3], func=AF.Sigmoid, scale=-1.0),
                             reads=[mm], writes=[mm])
                        b.op("act", lambda h: h.activation(out=mm[:, 4:5], in_=mm[:, 2:3], func=AF.Sigmoid),
                             reads=[mm], writes=[mm])
                        b.op("dve", lambda h, lt=lt: h.tensor_scalar(out=G[:, lt], in0=msk1[:], scalar1=mm[:, 3:4], scalar2=None,
                                                                     op0=ALU.mult), reads=[msk1, mm], writes=[G])
                        b.op("dve", lambda h, lt=lt: h.scalar_tensor_tensor(out=G[:, lt], in0=msk2[:], scalar=mm[:, 4:5],
                                                                            in1=G[:, lt], op0=ALU.mult, op1=ALU.add),
                             reads=[msk2, mm, G], writes=[G])
                    ps_T = nps()
                    for ts_ in range(4):
                        b.op("pe", lambda h, ts_=ts_: h.transpose(ps_T[0:8, ts_ * 128:(ts_ + 1) * 128], G[:, ts_ * 8:(ts_ + 1) * 8],
                                                                  ident[:, :]), reads=[G, ident], writes=[ps_T], inc=(ts_ == 3))
                    b.op("act", lambda h: h.copy(out=gT[:], in_=ps_T[0:8, :]), reads=[ps_T], writes=[gT])
                    for e in range(NE):
                        psb = nps()
                        b.op("pe", lambda h, e=e, psb=psb: h.matmul(psb[:, :], sel[:, e, :], gT[:, :], start=True, stop=True),
                             reads=[sel, gT], writes=[psb])
                        b.op("act", lambda h, e=e, psb=psb: h.copy(out=gatebc[:, e, csl], in_=psb[:, :]),
                             reads=[psb], writes=[gatebc])
            for bi in range(len(blocks)):
                n = s_ * len(blocks) + bi
                if n + nws - 1 < total_blocks:
                    load_block(n + nws - 1)
                e, fb = blocks[bi]
                sl = n % nws
                for sub in range(2):
                    csl = slice(sub * TT, (sub + 1) * TT)
                    x1 = x1s[sub]
                    hd_ = hid[sub]
                    for fc in range(4):
                        pg = nps()
                        for kc in range(8):
                            b.op("pe", lambda h, kc=kc, fc=fc, pg=pg: h.matmul(
                                pg[:, :], wgs[sl][:, kc, fc * 128:(fc + 1) * 128], h2[:, kc, csl], start=(kc == 0), stop=(kc == 7)),
                                reads=[wgs[sl], h2], writes=[pg], inc=(kc == 7))
                        pu = nps()
                        for kc in range(8):
                            b.op("pe", lambda h, kc=kc, fc=fc, pu=pu: h.matmul(
                                pu[:, :], wus[sl][:, kc, fc * 128:(fc + 1) * 128], h2[:, kc, csl], start=(kc == 0), stop=(kc == 7)),
                                reads=[wus[sl], h2], writes=[pu], inc=(kc == 7))
                        sgb = sg[fc % 2]
                        b.op("act", lambda h, pg=pg, sgb=sgb: h.activation(out=sgb[:], in_=pg[:, :], func=AF.Silu),
                             reads=[pg], writes=[sgb])
                        if moe:
                            b.op("pool", lambda h, sgb=sgb, e=e: h.tensor_tensor(out=sgb[:], in0=sgb[:], in1=gatebc[:, e, csl],
                                                                                 op=ALU.mult), reads=[sgb, gatebc], writes=[sgb])
                        b.op("dve", lambda h, fc=fc, pu=pu, sgb=sgb, hd_=hd_: h.tensor_tensor(
                            out=hd_[:, fc, :], in0=pu[:, :], in1=sgb[:], op=ALU.mult), reads=[pu, sgb], writes=[hd_])
                    for oc in range(8):
                        pd = nps()
                        for fc in range(4):
                            b.op("pe", lambda h, fc=fc, oc=oc, pd=pd, hd_=hd_: h.matmul(
                                pd[:, :], wds[sl][:, fc, oc * 128:(oc + 1) * 128], hd_[:, fc, :], start=(fc == 0), stop=(fc == 3)),
                                reads=[wds[sl], hd_], writes=[pd], inc=(fc == 3))
                        b.op("dve", lambda h, oc=oc, pd=pd, x1=x1: h.scalar_tensor_tensor(
                            out=x1[:, oc, :], in0=pd[:, :], scalar=g2[:, oc:oc + 1], in1=x1[:, oc, :],
                            op0=ALU.mult, op1=ALU.add), reads=[pd, g2, x1], writes=[x1])
            for sub in range(2):
                x1 = x1s[sub]
                if last:
                    emit_norm(b, nc, x1, slice(0, TT), nps(), ones_bf, sqb, rstd, xn, fg, None, x1, slice(0, TT), TT)
                b.dma("sp", outv[:, :, s_ * STK + sub * TT:s_ * STK + (sub + 1) * TT], x1[:], reads=[x1], sem=s_outs[sub],
                      is_out=last)
    b.end_phase()


def build_fused(ntiles=NT, nsuper_all=S // STK, nsuper_half=NTOK // STK):
    nc = bass.Bass("TRN2", target_bir_lowering=False)
    dr = lambda n, s, dt, k="ExternalInput": nc.dram_tensor(n, list(s), dt, kind=k).ap()
    d = {}
    d["xT"] = dr("xT", [D, S], F32)
    d["pos"] = dr("pos", [1, S], I32)
    d["cvec"] = dr("cvec", [128, 8], F32)
    d["selm"] = dr("selm", [128, 2], F32)
    d["sgn"] = dr("sgn", [128, 1], F32)
    for l in range(DEPTH):
        d["adaw%d" % l] = dr("adaw%d" % l, [D, 6 * D], F32)
        d["adab%d" % l] = dr("adab%d" % l, [128, 48], F32)
        d["lng1_%d" % l] = dr("lng1_%d" % l, [128, 8], F32)
        d["lng2_%d" % l] = dr("lng2_%d" % l, [128, 8], F32)
        d["lamv%d" % l] = dr("lamv%d" % l, [1, 256], F32)
        d["wout%d" % l] = dr("wout%d" % l, [D, D], F32)
        for hh in range(2):
            sfx = "%d_%d" % (l, hh)
            d["win" + sfx] = dr("win" + sfx, [D, 1792], F32)
            d["convw" + sfx] = dr("convw" + sfx, [128, 2, 4], F32)
            d["small" + sfx] = dr("small" + sfx, [128, 10], F32)
            d["gw" + sfx] = dr("gw" + sfx, [128, 4, 128], F32)
    d["wg0"] = dr("wg0", [1, D, DFF], F32)
    d["wu0"] = dr("wu0", [1, D, DFF], F32)
    d["wd0"] = dr("wd0", [1, DFF, D], F32)
    d["router"] = dr("router", [128, 8, 8], F32)
    d["wg1"] = dr("wg1", [NE, D, DFF], F32)
    d["wu1"] = dr("wu1", [NE, D, DFF], F32)
    d["wd1"] = dr("wd1", [NE, DFF, D], F32)
    d["fing"] = dr("fing", [128, 8], F32)
    out_d = dr("xoT", [D, NTOK], F32, "ExternalOutput")
    mixS = [dr("mixS%d" % l, [D, S], BF16, "Internal") for l in range(DEPTH)]
    x1S = dr("x1S", [D, S], F32, "Internal")
    fm = lambda ap: ap.rearrange("(c p) t -> p c t", p=128)

    with contextlib.ExitStack() as es:
        b = B(nc, es)
        P = [b.ps("P%d" % i) for i in range(8)]
        b.eps = b.sb("eps_c", [128, 1], F32)
        b.op("dve", lambda h: h.memset(b.eps[:], EPS), writes=[b.eps])
        b.cs = b.sb("cs_glob", [128, 8], F32)
        modalls = [b.sb("modall%d" % l, [128, 48], F32) for l in range(DEPTH)]
        for l in range(DEPTH):
            emit_mod(b, nc, P, "MOD%d_" % l, d["cvec"], d["adaw%d" % l], d["adab%d" % l], modalls[l], first=(l == 0))
            xv = fm(d["xT"]) if l == 0 else fm(x1S)
            for hh in range(2):
                sfx = "%d_%d" % (l, hh)
                dm = dict(modall=modalls[l], lng=d["lng1_%d" % l],
                          win=d["win" + sfx], convw=d["convw" + sfx], small=d["small" + sfx], sgn=d["sgn"],
                          gw=d["gw" + sfx], lamv=d["lamv%d" % l], pos=d["pos"])
                emit_M(b, nc, P, "M%s_" % sfx, l, dm, xv, fm(mixS[l]), [2 * hh, 2 * hh + 1, 4 + 2 * hh, 5 + 2 * hh],
                       ntiles=ntiles)
            moe = (l % 2 == 1)
            last = (l == DEPTH - 1)
            df = dict(modall=modalls[l], lng=d["lng2_%d" % l],
                      wout=d["wout%d" % l], wg=d["wg%d" % l], wu=d["wu%d" % l], wd=d["wd%d" % l],
                      router=d["router"], fing=d["fing"])
            if not last:
                emit_F(b, nc, P, "F%d_" % l, l, moe, last, df, [xv], [fm(mixS[l])], fm(x1S), nsuper_all)
            else:
                mv = fm(mixS[l])
                emit_F(b, nc, P, "F%d_" % l, l, moe, last, df, [xv[:, :, 0:NTOK], xv[:, :, NTOK:S]],
                       [mv[:, :, 0:NTOK], mv[:, :, NTOK:S]], fm(out_d), nsuper_half, selm=d["selm"])
        b.finish()
    return nc


def make_inputs(inp):
    f32 = np.float32
    per_l = []
    for l in range(DEPTH):
        shared, com = prep_M(inp, l)
        per_l.append((shared, com))
    maps = []
    for core in range(8):
        bb, hf = core // 2, core % 2
        m = dict(xT=np.ascontiguousarray(inp["x"][bb].T),
                 pos=np.ascontiguousarray(inp["positions"][bb][None, :]).astype(np.int32),
                 cvec=np.ascontiguousarray(inp["c"][bb].reshape(8, 128).T),
                 selm=np.tile(np.array([[1.0 - hf, float(hf)]], f32), (128, 1)),
                 sgn=per_l[0][1]["sgn"])
        for l in range(DEPTH):
            shared, com = per_l[l]
            m["adaw%d" % l] = inp["ada_w"][l]
            m["adab%d" % l] = np.ascontiguousarray(inp["ada_b"][l].reshape(48, 128).T)
            m["lng1_%d" % l] = com["lng"]
            m["lng2_%d" % l] = np.ascontiguousarray(inp["ln2_g"][l].reshape(8, 128).T)
            m["lamv%d" % l] = com["lamv"]
            m["wout%d" % l] = inp["w_out"][l]
            for hh in range(2):
                sfx = "%d_%d" % (l, hh)
                for k in ("win", "convw", "small", "gw"):
                    m[k + sfx] = shared[hh][k]
        m["wg0"] = inp["ffn_w_gate"]
        m["wu0"] = inp["ffn_w_up"]
        m["wd0"] = inp["ffn_w_down"]
        m["router"] = np.ascontiguousarray(inp["moe_router"][0].reshape(8, 128, 8).transpose(1, 0, 2))
        m["wg1"] = inp["moe_w_gate"][0]
        m["wu1"] = inp["moe_w_up"][0]
        m["wd1"] = inp["moe_w_down"][0]
        m["fing"] = np.ascontiguousarray(inp["final_g"].reshape(8, 128).T)
        maps.append(m)
    return maps


def kernel(**inp):
    inp = {k: np.asarray(v) for k, v in inp.items()}
    nc = build_fused()
    maps = make_inputs(inp)
    res = run_bass_kernel_spmd(nc, maps, core_ids=list(range(8)))
    out = np.empty((NB, S, D), np.float32)
    for core in range(8):
        bb, hf = core // 2, core % 2
        out[bb, hf * NTOK:(hf + 1) * NTOK, :] = np.asarray(res.results[core]["xoT"]).T
    return out
```
